# Optimizing a Trainium2 kernel written in Bass

```python
import numpy as np
import jax
import jax.numpy as jnp
from jax import lax

D_MODEL = 1024
BATCH = 16
SEQ = 2048
DEPTH = 2

HEAD_DIM = 64
ROPE_DIM = HEAD_DIM // 4
ROPE_THETA = 500000.0
NORM_EPS = 1e-6
ATTN_SCALE = HEAD_DIM ** -0.5
Q_BLOCK = 32

NSA_HEADS = 8
NSA_KV_GROUPS = 2
NSA_GROUP_SIZE = NSA_HEADS // NSA_KV_GROUPS
CMP_BLOCK = 32
CMP_STRIDE = 16
CMP_HIDDEN = 2 * HEAD_DIM
SLC_BLOCK = 64
N_SLC = 16
N_LOCAL_BLOCKS = 2
SEL_FORCE_SCORE = 1e4
WINDOW = 512

DSA_HEADS = 8
IDX_HEADS = 8
IDX_DIM = 64
IDX_WEIGHT_SCALE = (IDX_HEADS * IDX_DIM) ** -0.5
DSA_TOPK_MAX = 256

D_FF = -(-8 * D_MODEL // (3 * 256)) * 256

NSA_WIDTH = NSA_HEADS * HEAD_DIM
DSA_WIDTH = DSA_HEADS * HEAD_DIM
KV_WIDTH = NSA_KV_GROUPS * HEAD_DIM
IN_SIZES = (
    NSA_WIDTH,
    KV_WIDTH, KV_WIDTH,
    KV_WIDTH, KV_WIDTH,
    KV_WIDTH, KV_WIDTH,
    3 * NSA_HEADS,
    DSA_WIDTH, DSA_WIDTH, DSA_WIDTH,
    IDX_HEADS * IDX_DIM, IDX_DIM, IDX_HEADS,
    D_MODEL, D_MODEL,
)
D_IN = sum(IN_SIZES)

kernel_name = 'nsa_dsa_gated_hybrid_block'


def _rmsnorm(x, g):
    xf = x.astype(jnp.float32)
    y = xf * lax.rsqrt(jnp.mean(xf * xf, axis=-1, keepdims=True) + NORM_EPS)
    return (y * g.astype(jnp.float32)).astype(x.dtype)


def _rope_tables(seq_len):
    pos = jnp.arange(seq_len, dtype=jnp.float32)
    inv_freq = ROPE_THETA ** (-jnp.arange(0, ROPE_DIM, 2, dtype=jnp.float32) / ROPE_DIM)
    ang = pos[:, None] * inv_freq[None, :]
    return jnp.cos(ang), jnp.sin(ang)


def _apply_rope(x, cos, sin):
    half = ROPE_DIM // 2
    bshape = (cos.shape[0],) + (1,) * (x.ndim - 3) + (half,)
    c = cos.reshape(bshape).astype(x.dtype)
    s = sin.reshape(bshape).astype(x.dtype)
    x1 = x[..., :half]
    x2 = x[..., half:ROPE_DIM]
    return jnp.concatenate([x1 * c - x2 * s, x2 * c + x1 * s, x[..., ROPE_DIM:]], axis=-1)


def _masked_softmax(scores, mask):
    s = jnp.where(mask, scores.astype(jnp.float32), -1e30)
    s = s - jnp.max(s, axis=-1, keepdims=True)
    p = jnp.where(mask, jnp.exp(s), 0.0)
    return p / jnp.maximum(jnp.sum(p, axis=-1, keepdims=True), 1e-30)


def _compress(kv, pos_emb, w1, b1, w2, b2):
    bsz, seq_len, groups, hd = kv.shape
    n_cmp = (seq_len - CMP_BLOCK) // CMP_STRIDE + 1
    idx = np.arange(n_cmp)[:, None] * CMP_STRIDE + np.arange(CMP_BLOCK)[None, :]
    blocks = kv[:, idx] + pos_emb[:, None, :]
    blocks = blocks.transpose(0, 1, 3, 2, 4).reshape(bsz, n_cmp, groups, CMP_BLOCK * hd)
    hidden = jax.nn.gelu(blocks @ w1 + b1)
    return hidden @ w2 + b2


def _cmp_to_slc_overlap(n_cmp, n_slc):
    c_start = np.arange(n_cmp)[:, None] * CMP_STRIDE
    s_start = np.arange(n_slc)[None, :] * SLC_BLOCK
    ov = np.minimum(c_start + CMP_BLOCK, s_start + SLC_BLOCK) - np.maximum(c_start, s_start)
    return jnp.asarray(np.clip(ov, 0, None) / CMP_BLOCK, dtype=jnp.float32)


def _nsa_mixer(q, k_cmp, v_cmp, k_slc, v_slc, k_win, v_win, gate_logits, cos, sin,
               cmpk_pos, cmpk_w1, cmpk_b1, cmpk_w2, cmpk_b2,
               cmpv_pos, cmpv_w1, cmpv_b1, cmpv_w2, cmpv_b2):
    bsz, seq_len = q.shape[:2]
    G, R, hd = NSA_KV_GROUPS, NSA_GROUP_SIZE, HEAD_DIM
    kv_shape = (bsz, seq_len, G, hd)
    q = _apply_rope(q.reshape(bsz, seq_len, NSA_HEADS, hd), cos, sin)
    k_cmp = _apply_rope(k_cmp.reshape(kv_shape), cos, sin)
    k_slc = _apply_rope(k_slc.reshape(kv_shape), cos, sin)
    k_win = _apply_rope(k_win.reshape(kv_shape), cos, sin)
    v_slc = v_slc.reshape(kv_shape)
    v_win = v_win.reshape(kv_shape)

    kc = _compress(k_cmp, cmpk_pos, cmpk_w1, cmpk_b1, cmpk_w2, cmpk_b2)
    vc = _compress(v_cmp.reshape(kv_shape), cmpv_pos, cmpv_w1, cmpv_b1, cmpv_w2, cmpv_b2)
    n_cmp = kc.shape[1]
    n_slc = seq_len // SLC_BLOCK
    n_sel = min(N_SLC, n_slc)
    nqb = seq_len // Q_BLOCK
    cmp_end = jnp.asarray(np.arange(n_cmp) * CMP_STRIDE + CMP_BLOCK - 1)
    overlap = _cmp_to_slc_overlap(n_cmp, n_slc)
    blk_ids = jnp.arange(n_slc)

    ks_blk = k_slc.reshape(bsz, n_slc, SLC_BLOCK, G, hd).transpose(0, 3, 1, 2, 4)
    vs_blk = v_slc.reshape(bsz, n_slc, SLC_BLOCK, G, hd).transpose(0, 3, 1, 2, 4)
    kw_pad = jnp.pad(k_win, ((0, 0), (WINDOW, 0), (0, 0), (0, 0)))
    vw_pad = jnp.pad(v_win, ((0, 0), (WINDOW, 0), (0, 0), (0, 0)))
    qb = q.reshape(bsz, nqb, Q_BLOCK, G, R, hd).transpose(1, 0, 3, 4, 2, 5)
    gather_blocks = jax.vmap(jax.vmap(lambda blk, ix: blk[ix]))

    def step(args):
        q_i, i = args
        t = i * Q_BLOCK + jnp.arange(Q_BLOCK)
        s_c = jnp.einsum('bgrtd,bcgd->bgrtc', q_i, kc) * ATTN_SCALE
        p_c = _masked_softmax(s_c, cmp_end[None, :] <= t[:, None])
        o_c = jnp.einsum('bgrtc,bcgd->bgrtd', p_c.astype(vc.dtype), vc)
        imp = jnp.einsum('bgrtc,cj->bgtj', p_c, overlap)
        cur = t // SLC_BLOCK
        dist = cur[:, None] - blk_ids[None, :]
        forced = (blk_ids[None, :] == 0) | ((dist >= 0) & (dist < N_LOCAL_BLOCKS))
        admissible = blk_ids[None, :] * SLC_BLOCK <= t[:, None]
        score = jnp.where(forced, SEL_FORCE_SCORE, imp)
        score = jnp.where(admissible, score, -jnp.inf)
        _, sel = lax.top_k(score, n_sel)
        kg = gather_blocks(ks_blk, sel)
        vg = gather_blocks(vs_blk, sel).reshape(bsz, G, Q_BLOCK, n_sel * SLC_BLOCK, hd)
        tok = sel[..., None] * SLC_BLOCK + jnp.arange(SLC_BLOCK)
        mask_s = (tok <= t[None, None, :, None, None]).reshape(bsz, G, 1, Q_BLOCK, n_sel * SLC_BLOCK)
        s_s = jnp.einsum('bgrtd,bgtnkd->bgrtnk', q_i, kg).reshape(bsz, G, R, Q_BLOCK, n_sel * SLC_BLOCK)
        p_s = _masked_softmax(s_s * ATTN_SCALE, mask_s)
        o_s = jnp.einsum('bgrtm,bgtmd->bgrtd', p_s.astype(vg.dtype), vg)
        kw = lax.dynamic_slice_in_dim(kw_pad, i * Q_BLOCK, WINDOW + Q_BLOCK, axis=1)
        vw = lax.dynamic_slice_in_dim(vw_pad, i * Q_BLOCK, WINDOW + Q_BLOCK, axis=1)
        s_pos = i * Q_BLOCK - WINDOW + jnp.arange(WINDOW + Q_BLOCK)
        diff = t[:, None] - s_pos[None, :]
        mask_w = (diff >= 0) & (diff < WINDOW) & (s_pos[None, :] >= 0)
        s_w = jnp.einsum('bgrtd,bsgd->bgrts', q_i, kw) * ATTN_SCALE
        p_w = _masked_softmax(s_w, mask_w)
        o_w = jnp.einsum('bgrts,bsgd->bgrtd', p_w.astype(vw.dtype), vw)
        return o_c, o_s, o_w

    o_c, o_s, o_w = lax.map(step, (qb, jnp.arange(nqb)))

    def unblock(o):
        return o.transpose(1, 0, 4, 2, 3, 5).reshape(bsz, seq_len, NSA_HEADS, hd)

    g = jax.nn.sigmoid(gate_logits).reshape(bsz, seq_len, 3, NSA_HEADS)[..., None]
    out = g[:, :, 0] * unblock(o_c) + g[:, :, 1] * unblock(o_s) + g[:, :, 2] * unblock(o_w)
    return out.reshape(bsz, seq_len, NSA_WIDTH)


def _dsa_mixer(q, k, v, iq, ik, iw, cos, sin):
    bsz, seq_len = q.shape[:2]
    q = _apply_rope(q.reshape(bsz, seq_len, DSA_HEADS, HEAD_DIM), cos, sin)
    k = _apply_rope(k.reshape(bsz, seq_len, DSA_HEADS, HEAD_DIM), cos, sin)
    v = v.reshape(bsz, seq_len, DSA_HEADS, HEAD_DIM)
    iq = _apply_rope(iq.reshape(bsz, seq_len, IDX_HEADS, IDX_DIM), cos, sin)
    ik = _apply_rope(ik, cos, sin)
    iw = iw * IDX_WEIGHT_SCALE
    k_top = min(DSA_TOPK_MAX, seq_len // 4)
    nqb = seq_len // Q_BLOCK
    qb = q.reshape(bsz, nqb, Q_BLOCK, DSA_HEADS, HEAD_DIM).transpose(1, 0, 2, 3, 4)
    iqb = iq.reshape(bsz, nqb, Q_BLOCK, IDX_HEADS, IDX_DIM).transpose(1, 0, 2, 3, 4)
    iwb = iw.reshape(bsz, nqb, Q_BLOCK, IDX_HEADS).transpose(1, 0, 2, 3)
    key_pos = jnp.arange(seq_len)
    gather_tokens = jax.vmap(lambda a, ix: a[ix])

    def step(args):
        q_i, iq_i, iw_i, i = args
        t = i * Q_BLOCK + jnp.arange(Q_BLOCK)
        rel = jax.nn.relu(jnp.einsum('bthd,bsd->bths', iq_i, ik).astype(jnp.float32))
        score = jnp.einsum('bths,bth->bts', rel, iw_i.astype(jnp.float32))
        score = jnp.where(key_pos[None, :] <= t[:, None], score, -jnp.inf)
        _, sel = lax.top_k(score, k_top)
        kg = gather_tokens(k, sel)
        vg = gather_tokens(v, sel)
        valid = (sel <= t[None, :, None])[:, None]
        s = jnp.einsum('bthd,btkhd->bhtk', q_i, kg) * ATTN_SCALE
        p = _masked_softmax(s, valid)
        return jnp.einsum('bhtk,btkhd->bthd', p.astype(vg.dtype), vg)

    o = lax.map(step, (qb, iqb, iwb, jnp.arange(nqb)))
    return o.transpose(1, 0, 2, 3, 4).reshape(bsz, seq_len, DSA_WIDTH)


def _hybrid_mixer(h, cos, sin, w_in,
                  cmpk_pos, cmpk_w1, cmpk_b1, cmpk_w2, cmpk_b2,
                  cmpv_pos, cmpv_w1, cmpv_b1, cmpv_w2, cmpv_b2,
                  w_branch_nsa, w_branch_dsa, w_out):
    proj = h @ w_in
    points = [int(p) for p in np.cumsum(IN_SIZES)[:-1]]
    (nsa_q, k_cmp, v_cmp, k_slc, v_slc, k_win, v_win, nsa_gate,
     dsa_q, dsa_k, dsa_v, idx_q, idx_k, idx_w, gate_nsa, gate_dsa) = jnp.split(proj, points, axis=-1)
    y_nsa = _nsa_mixer(nsa_q, k_cmp, v_cmp, k_slc, v_slc, k_win, v_win, nsa_gate, cos, sin,
                       cmpk_pos, cmpk_w1, cmpk_b1, cmpk_w2, cmpk_b2,
                       cmpv_pos, cmpv_w1, cmpv_b1, cmpv_w2, cmpv_b2) @ w_branch_nsa
    y_dsa = _dsa_mixer(dsa_q, dsa_k, dsa_v, idx_q, idx_k, idx_w, cos, sin) @ w_branch_dsa
    merged = jax.nn.sigmoid(gate_nsa) * y_nsa + jax.nn.sigmoid(gate_dsa) * y_dsa
    return merged @ w_out


def _swiglu(h, w_gate, w_up, w_down):
    return (jax.nn.silu(h @ w_gate) * (h @ w_up)) @ w_down


def setup_inputs(seed: int = 0) -> dict:
    key = jax.random.key(seed)
    ks = jax.random.split(key, 21)

    def nrm(k, shape, scale):
        return jax.random.normal(k, shape, jnp.float32) * scale

    flat = CMP_BLOCK * HEAD_DIM
    return {
        'x': nrm(ks[0], (BATCH, SEQ, D_MODEL), 1.0),
        'attn_norm': 1.0 + nrm(ks[1], (DEPTH, D_MODEL), 0.01),
        'w_in': nrm(ks[2], (DEPTH, D_MODEL, D_IN), D_MODEL ** -0.5),
        'cmpk_pos': nrm(ks[3], (DEPTH, CMP_BLOCK, HEAD_DIM), 0.1),
        'cmpk_w1': nrm(ks[4], (DEPTH, flat, CMP_HIDDEN), flat ** -0.5),
        'cmpk_b1': nrm(ks[5], (DEPTH, CMP_HIDDEN), 0.01),
        'cmpk_w2': nrm(ks[6], (DEPTH, CMP_HIDDEN, HEAD_DIM), CMP_HIDDEN ** -0.5),
        'cmpk_b2': nrm(ks[7], (DEPTH, HEAD_DIM), 0.01),
        'cmpv_pos': nrm(ks[8], (DEPTH, CMP_BLOCK, HEAD_DIM), 0.1),
        'cmpv_w1': nrm(ks[9], (DEPTH, flat, CMP_HIDDEN), flat ** -0.5),
        'cmpv_b1': nrm(ks[10], (DEPTH, CMP_HIDDEN), 0.01),
        'cmpv_w2': nrm(ks[11], (DEPTH, CMP_HIDDEN, HEAD_DIM), CMP_HIDDEN ** -0.5),
        'cmpv_b2': nrm(ks[12], (DEPTH, HEAD_DIM), 0.01),
        'w_branch_nsa': nrm(ks[13], (DEPTH, NSA_WIDTH, D_MODEL), NSA_WIDTH ** -0.5),
        'w_branch_dsa': nrm(ks[14], (DEPTH, DSA_WIDTH, D_MODEL), DSA_WIDTH ** -0.5),
        'w_out': nrm(ks[15], (DEPTH, D_MODEL, D_MODEL), D_MODEL ** -0.5),
        'ffn_norm': 1.0 + nrm(ks[16], (DEPTH, D_MODEL), 0.01),
        'w_ffn_gate': nrm(ks[17], (DEPTH, D_MODEL, D_FF), D_MODEL ** -0.5),
        'w_ffn_up': nrm(ks[18], (DEPTH, D_MODEL, D_FF), D_MODEL ** -0.5),
        'w_ffn_down': nrm(ks[19], (DEPTH, D_FF, D_MODEL), D_FF ** -0.5),
        'final_norm': 1.0 + nrm(ks[20], (D_MODEL,), 0.01),
    }


def reference(x, attn_norm, w_in,
              cmpk_pos, cmpk_w1, cmpk_b1, cmpk_w2, cmpk_b2,
              cmpv_pos, cmpv_w1, cmpv_b1, cmpv_w2, cmpv_b2,
              w_branch_nsa, w_branch_dsa, w_out,
              ffn_norm, w_ffn_gate, w_ffn_up, w_ffn_down, final_norm):
    cos, sin = _rope_tables(x.shape[1])
    for l in range(DEPTH):
        h = _rmsnorm(x, attn_norm[l])
        x = x + _hybrid_mixer(h, cos, sin, w_in[l],
                              cmpk_pos[l], cmpk_w1[l], cmpk_b1[l], cmpk_w2[l], cmpk_b2[l],
                              cmpv_pos[l], cmpv_w1[l], cmpv_b1[l], cmpv_w2[l], cmpv_b2[l],
                              w_branch_nsa[l], w_branch_dsa[l], w_out[l])
        h = _rmsnorm(x, ffn_norm[l])
        x = x + _swiglu(h, w_ffn_gate[l], w_ffn_up[l], w_ffn_down[l])
    return _rmsnorm(x, final_norm)
```

```python
import os
import numpy as np
from contextlib import ExitStack
import ml_dtypes
import concourse.bass as bass
import concourse.mybir as mybir
from concourse.bass_utils import run_bass_kernel_spmd

F32 = mybir.dt.float32
BF16 = mybir.dt.bfloat16
AF = mybir.ActivationFunctionType
ALU = mybir.AluOpType
AX = mybir.AxisListType

S = 2048
D = 1024
TT = 16
DIN = 5472
DFF = 2816
NFC = 22
EPS = 1e-6
SCALE = 0.125
IDXS = 512.0 ** -0.5
BIG = 30000.0
NEG = -1e30
N_ITERS = 14


class T:
    __slots__ = ("h", "name", "writer", "readers", "dsem", "dcount", "excl", "eph")

    def __init__(self, h, name):
        self.h = h
        self.name = name
        self.writer = None
        self.readers = {}
        self.dsem = None
        self.dcount = 0
        self.excl = False
        self.eph = False

    def __getitem__(self, idx):
        return self.h[idx]


class Eng:
    def __init__(self, name, sem):
        self.name = name
        self.sem = sem
        self.count = 0
        self.known = {}
        self.prog = []


class Prog:
    def __init__(self, nc, stack):
        self.nc = nc
        self.stack = stack
        self.engs = {}
        for n in ("pe", "act", "dve", "pool", "sp"):
            sem = stack.enter_context(nc.semaphore("s_" + n))
            self.engs[n] = Eng(n, sem)
        self.ninstr = 0
        self.uid = 0
        self.dtiles = []

    def sb(self, shape, dtype, name="t"):
        self.uid += 1
        name = "%s_%d" % (name, self.uid)
        h = self.stack.enter_context(self.nc.sbuf_tensor(name, list(shape), dtype))
        return T(h, name)

    def ps(self, shape, dtype=F32, name="p"):
        self.uid += 1
        name = "%s_%d" % (name, self.uid)
        h = self.stack.enter_context(self.nc.psum_tensor(name, list(shape), dtype))
        t = T(h, name)
        t.excl = True
        return t

    def dummy(self, name):
        return T(None, name)

    def _dsem(self, t, q="sp"):
        kind = "sw" if q == "pool" else "hw"
        if t.dsem is None:
            pool = getattr(self, "sem_pool", {}).get(kind)
            if t.eph and pool:
                t.dsem, t.dcount = pool.pop()
            else:
                t.dsem = self.stack.enter_context(self.nc.semaphore("d_" + t.name))
            t.name = t.name + ":" + kind
            self.dtiles.append(t)
        else:
            assert t.name.endswith(":" + kind), "mixed DMA kinds on tile %s" % t.name
        return t.dsem

    def _deps(self, E, reads, writes):
        need = {}
        own = id(E.sem)
        is_pe = E.name == "pe"

        def add(sv, raw):
            if sv is None:
                return
            s, v = sv
            k = id(s)
            if k == own and is_pe:
                return
            if E.known.get(k, 0) >= v:
                return
            if k not in need or need[k][1] < v:
                need[k] = (s, v)

        for t in reads:
            add(t.writer, True)
            if t.excl:
                for sv in t.readers.values():
                    if sv[0] is not E.sem:
                        add(sv, False)
        for t in writes:
            add(t.writer, False)
            for sv in t.readers.values():
                add(sv, False)
        for k, (s, v) in need.items():
            E.known[k] = v
        return list(need.values())

    def op(self, eng, fn, reads=(), writes=()):
        E = self.engs[eng]
        waits = self._deps(E, reads, writes)
        E.count += 1
        sv = (E.sem, E.count)
        for t in reads:
            t.readers[id(E.sem)] = sv
        for t in writes:
            t.writer = sv
            t.readers = {}
        sem = E.sem

        def run(e, waits=waits, fn=fn, sem=sem):
            for (s, v) in waits:
                e.wait_ge(s, v)
            fn(e).then_inc(sem, 1)
        E.prog.append(run)
        self.ninstr += 1

    def dma(self, q, out_ap, in_ap, reads=(), writes=(), **kw):
        E = self.engs[q]
        waits = self._deps(E, reads, writes)
        owner = writes[0] if writes else reads[0]
        sem = self._dsem(owner, q)
        owner.dcount += 16
        sv = (sem, owner.dcount)
        for t in reads:
            t.readers[id(sem)] = sv
        for t in writes:
            t.writer = sv
            t.readers = {}

        def run(e, waits=waits, sem=sem, out_ap=out_ap, in_ap=in_ap, kw=kw):
            for (s, v) in waits:
                e.wait_ge(s, v)
            e.dma_start(out=out_ap, in_=in_ap, **kw).then_inc(sem, 16)
        E.prog.append(run)
        self.ninstr += 1

    def barrier(self):
        front = [(E.sem, E.count) for E in self.engs.values() if E.count > 0]
        front += [(t.dsem, t.dcount) for t in self.dtiles if t.dcount > 0]
        for E in self.engs.values():
            waits = []
            for (s, v) in front:
                if s is E.sem:
                    continue
                if E.known.get(id(s), 0) >= v:
                    continue
                E.known[id(s)] = v
                waits.append((s, v))

            def run(e, waits=waits):
                for (s, v) in waits:
                    e.wait_ge(s, v)
            E.prog.append(run)
        if not hasattr(self, "sem_pool"):
            self.sem_pool = {"sw": [], "hw": []}
        keep = []
        for t in self.dtiles:
            if t.eph:
                self.sem_pool[t.name[-2:]].append((t.dsem, t.dcount))
            else:
                keep.append(t)
        self.dtiles = keep

    def build(self):
        nc = self.nc
        with nc.Block() as block:
            @block.tensor
            def _(e):
                for f in self.engs["pe"].prog:
                    f(e)

            @block.scalar
            def _(e):
                for f in self.engs["act"].prog:
                    f(e)

            @block.vector
            def _(e):
                for f in self.engs["dve"].prog:
                    f(e)

            @block.gpsimd
            def _(e):
                for f in self.engs["pool"].prog:
                    f(e)

            @block.sync
            def _(e):
                for f in self.engs["sp"].prog:
                    f(e)


class Arena:
    def __init__(self, P, nbytes):
        self.P = P
        self.t = P.sb([128, nbytes // 4], F32, "arena")
        self.off = 0
        self.cap = nbytes
        self.n = 0

    def alloc(self, parts, free, dtype, name="a"):
        n = int(np.prod(free))
        esz = 4 if dtype == F32 else 2
        nb = (n * esz + 63) // 64 * 64
        assert self.off + nb <= self.cap, "arena overflow %s %d" % (name, self.off + nb)
        ap = self.t.h[0:parts, self.off // 4:(self.off + nb) // 4]
        if dtype != F32:
            ap = ap.bitcast(dtype)
        ap = ap[:, 0:n]
        if len(free) == 2:
            ap = ap.rearrange("p (a b) -> p a b", a=free[0])
        elif len(free) == 3:
            ap = ap.rearrange("p (a b c) -> p a b c", a=free[0], b=free[1])
        self.off += nb
        self.n += 1
        t = T(ap, "%s%d" % (name, self.n))
        t.eph = True
        return t


def host_consts():
    bf = ml_dtypes.bfloat16
    c = {}
    c["c_ident"] = np.eye(128, dtype=np.float32).astype(bf)
    c["c_identbig"] = (np.eye(128, dtype=np.float32) * BIG).astype(bf)
    pos = np.arange(S, dtype=np.float32)
    inv = (500000.0 ** (-np.arange(0, 16, 2, dtype=np.float32) / 16)).astype(np.float32)
    ang = pos[:, None] * inv[None, :]
    cos = np.cos(ang).astype(np.float32)
    sin = np.sin(ang).astype(np.float32)
    c["c_cos2"] = np.concatenate([cos, cos], 1).astype(np.float32)
    c["c_sin2"] = np.concatenate([-sin, sin], 1).astype(np.float32)
    k = np.arange(128)[:, None]
    q = np.arange(128)[None, :]
    c["c_tri"] = np.where(k <= q, 0.0, -1.0).astype(bf)
    c["c_anti"] = np.where(k > q, 0.0, -1.0).astype(bf)
    c["c_triT"] = np.where(q <= k, 0.0, NEG).astype(np.float32)
    j = np.arange(32)[:, None]
    kk = np.arange(S)[None, :]
    c["c_E"] = (kk // 64 == j).astype(np.float32).astype(bf)
    cc = np.arange(127)[:, None]
    c["c_cmask"] = np.where(16 * cc + 31 <= kk, 0.0, -1.0).astype(bf)
    c_start = np.arange(127)[:, None] * 16
    s_start = np.arange(32)[None, :] * 64
    ov = np.minimum(c_start + 32, s_start + 64) - np.maximum(c_start, s_start)
    c["c_ov"] = (np.clip(ov, 0, None) / 32.0).astype(np.float32).astype(bf)
    t = np.arange(S)[:, None]
    jj = np.arange(32)[None, :]
    cur = t // 64
    dist = cur - jj
    forced = (jj == 0) | ((dist >= 0) & (dist < 2))
    adm = jj * 64 <= t
    c["c_mimp"] = (adm & ~forced).astype(np.float32)
    c["c_fbias"] = np.where(adm, np.where(forced, 1e4, 0.0), NEG).astype(np.float32)
    return c


def proj_chunks():
    ch = []
    ch.append((0, 512, [(i * 128, 128, "T", True, ("qnT", 2 * i)) for i in range(4)]))
    ch.append((512, 512, [(0, 128, "T", True, ("kcT", 0)), (128, 128, "T", False, ("vcT", 0)),
                          (256, 128, "T", True, ("ksT", 0)), (384, 128, "tok", False, ("vs", 0))]))
    ch.append((1024, 280, [(0, 128, "T", True, ("kwT", 0)), (128, 128, "tok", False, ("vw", 0)),
                           (256, 24, "sig", False, ("g24", 0))]))
    ch.append((1304, 512, [(i * 128, 128, "T", True, ("qdT", 2 * i)) for i in range(4)]))
    ch.append((1816, 512, [(i * 128, 128, "T", True, ("kdT", 2 * i)) for i in range(4)]))
    ch.append((2328, 512, [(i * 128, 128, "tok", False, ("vd", 2 * i)) for i in range(4)]))
    ch.append((2840, 512, [(i * 128, 128, "T", True, ("iqT", 2 * i)) for i in range(4)]))
    ch.append((3352, 72, [(0, 64, "T", True, ("ikT", 0)), (64, 8, "scale", False, ("iw", 0))]))
    return ch


class Builder:
    def __init__(self, nb, nl, dbg=False, phases=None):
        self.nb = nb
        self.nl = nl
        self.dbg = dbg
        self.phases = phases
        self.nc = bass.Bass("TRN2", target_bir_lowering=False)
        self.st = ExitStack()

    def din(self, name, shape, dtype=F32):
        return self.nc.dram_tensor(name, list(shape), dtype, kind="ExternalInput").ap()

    def dscr(self, name, shape, dtype):
        kind = "ExternalOutput" if self.dbg else "Internal"
        ap = self.nc.dram_tensor(name, list(shape), dtype, kind=kind).ap()
        self.scr[name] = ap
        self.scrT[name] = self.P.dummy(name)
        return ap

    def build(self):
        nc = self.nc
        nb, nl = self.nb, self.nl
        with self.st:
            P = self.P = Prog(nc, self.st)
            self.scr = {}
            self.scrT = {}
            self.x_in = self.din("x", [nb, S, D])
            self.w = {}
            for name, shape in [("attn_norm", [nl, D]), ("w_in", [nl, D, DIN]),
                                ("cmpk_pos", [nl, 32, 64]), ("cmpk_w1", [nl, 2048, 128]), ("cmpk_b1", [nl, 128]),
                                ("cmpk_w2", [nl, 128, 64]), ("cmpk_b2", [nl, 64]),
                                ("cmpv_pos", [nl, 32, 64]), ("cmpv_w1", [nl, 2048, 128]), ("cmpv_b1", [nl, 128]),
                                ("cmpv_w2", [nl, 128, 64]), ("cmpv_b2", [nl, 64]),
                                ("w_branch_nsa", [nl, 512, D]), ("w_branch_dsa", [nl, 512, D]), ("w_out", [nl, D, D]),
                                ("ffn_norm", [nl, D]), ("w_ffn_gate", [nl, D, DFF]), ("w_ffn_up", [nl, D, DFF]),
                                ("w_ffn_down", [nl, DFF, D]), ("final_norm", [D])]:
                self.w[name] = self.din(name, shape)
            hc = host_consts()
            self.cin = {}
            for k, v in hc.items():
                self.cin[k] = self.din(k, v.shape, BF16 if v.dtype == ml_dtypes.bfloat16 else F32)
            self.y_out = nc.dram_tensor("y", [nb, S, D], F32, kind="ExternalOutput").ap()
            self.dscr("xs", [S, D], F32)
            for nm, nh in [("qnT", 8), ("kcT", 2), ("vcT", 2), ("ksT", 2), ("kwT", 2), ("qdT", 8), ("kdT", 8), ("iqT", 8), ("ikT", 1)]:
                self.dscr(nm, [nh, 64, S], BF16)
            self.dscr("vs", [S, 128], BF16)
            self.dscr("vw", [S, 128], BF16)
            self.dscr("vd", [S, 512], BF16)
            self.dscr("g24", [S, 24], F32)
            self.dscr("iw", [S, 8], F32)
            self.xsT = [P.dummy("xs%d" % i) for i in range(TT)]
            self.PS = [P.ps([128, 512], F32, "bank") for _ in range(8)]
            self.ident = P.sb([128, 128], BF16, "ident")
            self.identbig = P.sb([128, 128], BF16, "identbig")
            self.cos2 = P.sb([128, TT, 16], F32, "cos2")
            self.sin2 = P.sb([128, TT, 16], F32, "sin2")
            self.tz = P.sb([128, 512], BF16, "tz")
            self.za = P.sb([128, 512], BF16, "za")
            self.gvec = P.sb([128, D], F32, "gvec")
            self.zeros = P.sb([128, 512], BF16, "zeros")
            P.op("pool", lambda e: e.memset(self.zeros[:], 0.0), [], [self.zeros])
            self.hT = [P.sb([128, 8, 512], BF16, "hT") for _ in range(4)]
            self.onT = P.sb([128, 4, S], BF16, "onT")
            self.odT = P.sb([128, 4, S], BF16, "odT")
            self.ar = Arena(P, 128 * 1024)
            P.dma("sp", self.ident[:], self.cin["c_ident"], writes=[self.ident])
            P.dma("sp", self.identbig[:], self.cin["c_identbig"], writes=[self.identbig])
            self.dma_tok(self.cos2, self.cin["c_cos2"])
            self.dma_tok(self.sin2, self.cin["c_sin2"])
            P.op("pool", lambda e: e.memset(self.tz[:], 0.0), [], [self.tz])
            P.op("pool", lambda e: e.memset(self.za[:], 0.0), [], [self.za])
            P.dma("sp", self.tz[:, 0:128], self.cin["c_tri"], writes=[self.tz])
            P.dma("sp", self.za[:, 384:512], self.cin["c_anti"], writes=[self.za])

            for b in range(nb):
                for l in range(nl):
                    self.layer(b, l)
                if self.want("F"):
                    self.phase_final(b)
                    P.barrier()
            P.barrier()
            P.build()
        return nc

    def want(self, ph):
        return self.phases is None or ph in self.phases

    def layer(self, b, l):
        P = self.P
        src = self.x_in[b] if l == 0 else self.scr["xs"]
        srcT = None if l == 0 else self.xsT
        if self.want("A") or self.want("a"):
            self.phase_norm(src, srcT, self.w["attn_norm"][l])
            if self.want("A"):
                self.phase_proj(l)
            P.barrier()
        if self.want("N"):
            self.phase_nsa(l)
            P.barrier()
        else:
            P.op("pool", lambda e: e.memset(self.onT[:], 0.0), [], [self.onT])
        if self.want("D"):
            self.phase_dsa(l)
            P.barrier()
        else:
            P.op("pool", lambda e: e.memset(self.odT[:], 0.0), [], [self.odT])
        if self.want("M"):
            self.phase_merge(b, l, src, srcT)
            P.barrier()
        if self.want("F"):
            self.phase_norm(self.scr["xs"], self.xsT, self.w["ffn_norm"][l])
            self.phase_ffn(b, l)
            P.barrier()

    def phase_norm(self, src, srcT, gdram):
        P = self.P
        ar = self.ar
        ar.off = 0
        P.dma("sp", self.gvec[:], gdram.partition_broadcast(128), writes=[self.gvec])
        xt = [ar.alloc(128, [D], F32, "xt") for _ in range(3)]
        hn = [ar.alloc(128, [D], BF16, "hn") for _ in range(2)]
        junk = ar.alloc(128, [D], BF16, "junk")
        st = [ar.alloc(128, [4], F32, "st") for _ in range(2)]
        for i in range(TT):
            x = xt[i % 3]
            rd = [srcT[i]] if srcT is not None else []
            P.dma("sp", x[:], src[i * 128:(i + 1) * 128, :], reads=rd, writes=[x])
            s = st[i % 2]
            h = hn[i % 2]
            P.op("act", lambda e, x=x, s=s: e.activation(out=junk[:], in_=x[:], func=AF.Square, accum_out=s[:, 0:1]), [x], [junk, s])
            P.op("dve", lambda e, s=s: e.tensor_scalar(out=s[:, 1:2], in0=s[:, 0:1], scalar1=1.0 / D, scalar2=EPS, op0=ALU.mult, op1=ALU.add), [s], [s])
            P.op("act", lambda e, s=s: e.activation(out=s[:, 2:3], in_=s[:, 1:2], func=AF.Sqrt), [s], [s])
            P.op("dve", lambda e, s=s: e.reciprocal(out=s[:, 3:4], in_=s[:, 2:3]), [s], [s])
            P.op("dve", lambda e, x=x, s=s, h=h: e.scalar_tensor_tensor(out=h[:], in0=x[:], scalar=s[:, 3:4], in1=self.gvec[:], op0=ALU.mult, op1=ALU.mult), [x, s, self.gvec], [h])
            pb = self.PS[4 + (i % 2)]
            pbv = pb.h.bitcast(BF16)
            for kc in range(8):
                P.op("pe", lambda e, h=h, kc=kc, pbv=pbv: e.transpose(pbv[:, kc * 128:(kc + 1) * 128], h[:, kc * 128:(kc + 1) * 128], self.ident[:]), [h, self.ident], [pb])
            ht = self.hT[i // 4]
            eng = "act" if i % 2 == 0 else "dve"
            dst = ht[:, :, (i % 4) * 128:(i % 4 + 1) * 128]
            srcv = pbv[:, 0:1024].rearrange("p (k t) -> p k t", k=8)
            if eng == "act":
                P.op("act", lambda e, dst=dst, srcv=srcv: e.copy(out=dst, in_=srcv), [pb], [ht])
            else:
                P.op("dve", lambda e, dst=dst, srcv=srcv: e.tensor_copy(out=dst, in_=srcv), [pb], [ht])

    def load_w(self, dst, src_ap, kparts=128):
        self.P.dma("pool", dst[:], src_ap.rearrange("(kc p) c -> p kc c", p=kparts), writes=[dst])

    def phase_proj(self, l):
        P = self.P
        ar = self.ar
        wbuf = [ar.alloc(128, [8, 512], BF16, "wb") for _ in range(3)]
        tokb = [ar.alloc(128, [512], BF16, "tokb") for _ in range(3)]
        t1 = [ar.alloc(128, [8, 16], F32, "t1") for _ in range(2)]
        t2 = [ar.alloc(128, [8, 16], F32, "t2") for _ in range(2)]
        stg = [ar.alloc(128, [4, 512], BF16, "stg") for _ in range(2)]
        sm = [ar.alloc(128, [32], F32, "sm") for _ in range(2)]
        chunks = proj_chunks()
        win = self.w["w_in"][l]
        cnt = 0
        import os
        sel = os.environ.get('KCH')
        def load_w(ci):
            c0, nc_, blocks = chunks[ci]
            wb = wbuf[ci % 3]
            P.dma("pool", wb[:, :, 0:nc_], win[:, c0:c0 + nc_].rearrange("(kc p) c -> p kc c", p=128), writes=[wb])

        def item(ci, i, cnt):
            c0, nc_, blocks = chunks[ci]
            wb = wbuf[ci % 3]
            if i == 0:
                if ci == 0:
                    load_w(0)
                if ci + 1 < len(chunks):
                    load_w(ci + 1)
            if True:
                ps = self.PS[cnt % 2]
                tb = tokb[cnt % 3]
                a1 = t1[cnt % 2]
                a2 = t2[cnt % 2]
                sg = stg[(i // 4) % 2]
                smt = sm[cnt % 2]
                cnt += 1
                ht = self.hT[i // 4]
                for kc in range(8):
                    P.op("pe", lambda e, ps=ps, ht=ht, kc=kc, i=i, wb=wb, nc_=nc_: e.matmul(ps[:, 0:nc_], lhsT=ht[:, kc, (i % 4) * 128:(i % 4 + 1) * 128], rhs=wb[:, kc, 0:nc_], start=(kc == 0), stop=(kc == 7)), [ht, wb], [ps])
                P.op("act", lambda e, tb=tb, ps=ps, nc_=nc_: e.copy(out=tb[:, 0:nc_], in_=ps[:, 0:nc_]), [ps], [tb])
                rblocks = [blk for blk in blocks if blk[3]]
                if rblocks and not os.environ.get('KNOROPE'):
                    nhr = (rblocks[-1][0] + rblocks[-1][1]) // 64
                    pv = ps[:, 0:nhr * 64].rearrange("p (h d) -> p h d", d=64)
                    cosb = self.cos2[:, i, :].unsqueeze(1).to_broadcast([128, nhr, 16])
                    sinlo = self.sin2[:, i, 0:8].unsqueeze(1).to_broadcast([128, nhr, 8])
                    sinhi = self.sin2[:, i, 8:16].unsqueeze(1).to_broadcast([128, nhr, 8])
                    P.op("dve", lambda e, a1=a1, pv=pv, cosb=cosb, nhr=nhr: e.tensor_tensor(out=a1[:, 0:nhr, :], in0=pv[:, :, 0:16], in1=cosb, op=ALU.mult), [ps, self.cos2], [a1])
                    P.op("dve", lambda e, a2=a2, pv=pv, sinlo=sinlo, nhr=nhr: e.tensor_tensor(out=a2[:, 0:nhr, 0:8], in0=pv[:, :, 8:16], in1=sinlo, op=ALU.mult), [ps, self.sin2], [a2])
                    P.op("dve", lambda e, a2=a2, pv=pv, sinhi=sinhi, nhr=nhr: e.tensor_tensor(out=a2[:, 0:nhr, 8:16], in0=pv[:, :, 0:8], in1=sinhi, op=ALU.mult), [ps, self.sin2], [a2])
                    runs = []
                    for (off, wdt, kind, rope, dest) in rblocks:
                        h0, h1 = off // 64, (off + wdt) // 64
                        if runs and runs[-1][1] == h0:
                            runs[-1][1] = h1
                        else:
                            runs.append([h0, h1])
                    for (h0, h1) in runs:
                        tv = tb[:, h0 * 64:h1 * 64].rearrange("p (h d) -> p h d", d=64)
                        P.op("dve", lambda e, a1=a1, a2=a2, tv=tv, h0=h0, h1=h1: e.tensor_tensor(out=tv[:, :, 0:16], in0=a1[:, h0:h1, :], in1=a2[:, h0:h1, :], op=ALU.add), [a1, a2, tb], [tb])
                yield
                pt = self.PS[2 + (cnt % 2)]
                ptv = pt.h.bitcast(BF16)
                nT = 0
                for bi, (off, wdt, kind, rope, dest) in enumerate(blocks):
                    dn, dh = dest
                    if kind == "T" and os.environ.get('KNOT'):
                        pass
                    elif kind == "T":
                        P.op("pe", lambda e, ptv=ptv, bi=bi, tb=tb, off=off, wdt=wdt: e.transpose(ptv[0:wdt, bi * 128:(bi + 1) * 128], tb[:, off:off + wdt], self.ident[:]), [tb, self.ident], [pt])
                        nT += 1
                    elif kind == "tok" and not os.environ.get('KNOOUT'):
                        P.dma("sp", self.scr[dn][i * 128:(i + 1) * 128, dh * 64:dh * 64 + wdt], tb[:, off:off + wdt], reads=[tb], writes=[self.scrT[dn]])
                    elif kind == "sig":
                        P.op("act", lambda e, smt=smt, ps=ps, off=off, wdt=wdt: e.activation(out=smt[:, 0:wdt], in_=ps[:, off:off + wdt], func=AF.Sigmoid), [ps], [smt])
                        P.dma("sp", self.scr[dn][i * 128:(i + 1) * 128, :], smt[:, 0:wdt], reads=[smt], writes=[self.scrT[dn]])
                    elif kind == "scale":
                        P.op("dve", lambda e, smt=smt, ps=ps, off=off, wdt=wdt: e.tensor_scalar(out=smt[:, 0:wdt], in0=ps[:, off:off + wdt], scalar1=IDXS, scalar2=None, op0=ALU.mult), [ps], [smt])
                        P.dma("sp", self.scr[dn][i * 128:(i + 1) * 128, :], smt[:, 0:wdt], reads=[smt], writes=[self.scrT[dn]])
                tblocks = [(bi, blk) for bi, blk in enumerate(blocks) if blk[2] == "T"]
                if tblocks and not os.environ.get('KNOT'):
                    full = all(blk[1] == 128 for _, blk in tblocks)
                    b0 = tblocks[0][0]
                    b1 = tblocks[-1][0] + 1
                    contiguous = [bi for bi, _ in tblocks] == list(range(b0, b1))
                    if full and contiguous:
                        srcv = ptv[:, b0 * 128:b1 * 128].rearrange("p (b t) -> p b t", t=128)
                        dstv = sg[:, b0:b1, (i % 4) * 128:(i % 4 + 1) * 128]
                        P.op("dve", lambda e, dstv=dstv, srcv=srcv: e.tensor_copy(out=dstv, in_=srcv), [pt], [sg])
                    else:
                        for bi, blk in tblocks:
                            wdt = blk[1]
                            P.op("dve", lambda e, sg=sg, ptv=ptv, bi=bi, wdt=wdt, i=i: e.tensor_copy(out=sg[0:wdt, bi, (i % 4) * 128:(i % 4 + 1) * 128], in_=ptv[0:wdt, bi * 128:(bi + 1) * 128]), [pt], [sg])
                    if i % 4 == 3:
                        tc = i // 4
                        for bi, blk in tblocks:
                            off, wdt, kind, rope, (dn, dh) = blk
                            nh = max(wdt // 64, 1)
                            dst = self.scr[dn][dh:dh + nh, :, tc * 512:(tc + 1) * 512].rearrange("h d t -> (h d) t")
                            P.dma("sp", dst, sg[0:wdt, bi, :], reads=[sg], writes=[self.scrT[dn]])


        items = []
        k = 0
        for ci in range(len(chunks)):
            for i in range(TT):
                items.append(item(ci, i, k))
                k += 1
        for k in range(len(items)):
            next(items[k])
            if k >= 1:
                for _ in items[k - 1]:
                    pass
        for _ in items[-1]:
            pass

    def phase_nsa(self, l):
        P = self.P
        ar = self.ar
        ar.off = 0
        qn = ar.alloc(96, [8, S], BF16, "qn")
        onsa = ar.alloc(128, [TT, 512], F32, "onsa")
        kcc = ar.alloc(64, [2, 128], BF16, "kcc")
        vca = ar.alloc(127, [2, 97], BF16, "vca")
        g24 = ar.alloc(128, [TT, 24], F32, "g24")
        self.cmask = ar.alloc(127, [S], BF16, "cmask")
        self.mimp = ar.alloc(128, [TT, 32], F32, "mimp")
        self.fbias = ar.alloc(128, [TT, 32], F32, "fbias")
        P.dma("sp", self.cmask[:], self.cin["c_cmask"], writes=[self.cmask])
        self.dma_tok(self.mimp, self.cin["c_mimp"])
        self.dma_tok(self.fbias, self.cin["c_fbias"])
        mark = ar.off
        for h in range(8):
            P.dma("sp", qn[0:64, h, :], self.scr["qnT"][h], reads=[self.scrT["qnT"]], writes=[qn])
        self.dma_tok(g24, self.scr["g24"], reads=[self.scrT["g24"]])
        P.op("pool", lambda e: e.memset(vca[:], 1.0), [], [vca])
        for g in range(2):
            P.dma("sp", vca[:, g, 65:97], self.cin["c_ov"], writes=[vca])
        kvT = ar.alloc(64, [2, S], BF16, "kvT")
        w1 = ar.alloc(64, [32, 128], BF16, "w1")
        w2 = ar.alloc(128, [64], BF16, "w2")
        posT = ar.alloc(64, [32], BF16, "posT")
        posn = ar.alloc(32, [64], BF16, "posn")
        b1 = ar.alloc(128, [1], F32, "b1")
        b2c = ar.alloc(64, [1], F32, "b2c")
        b2r = ar.alloc(127, [64], F32, "b2r")
        cb = ar.alloc(128, [2], F32, "cb")
        xs_ = ar.alloc(128, [128], F32, "gx")
        x2 = ar.alloc(128, [128], F32, "gx2")
        th = ar.alloc(128, [128], F32, "gth")
        gl = ar.alloc(128, [128], BF16, "gl")
        for kind in ("k", "v"):
            pre = "cmp%s_" % kind
            P.dma("sp", kvT[:], self.scr["kcT" if kind == "k" else "vcT"].rearrange("g d t -> d g t"), reads=[self.scrT["kcT" if kind == "k" else "vcT"]], writes=[kvT])
            P.dma("pool", w1[:], self.w[pre + "w1"][l].rearrange("(j d) h -> d j h", d=64), writes=[w1])
            P.dma("pool", w2[:], self.w[pre + "w2"][l], writes=[w2])
            P.dma("pool", posn[:], self.w[pre + "pos"][l], writes=[posn])
            ppos = self.PS[7]
            pposv = ppos.h.bitcast(BF16)
            P.op("pe", lambda e, pposv=pposv: e.transpose(pposv[0:64, 0:32], posn[:], self.ident[0:32, 0:32]), [posn, self.ident], [ppos])
            P.op("act", lambda e, pposv=pposv: e.copy(out=posT[:], in_=pposv[0:64, 0:32]), [ppos], [posT])
            P.dma("sp", b1[:], self.w[pre + "b1"][l].rearrange("(h o) -> h o", o=1), writes=[b1])
            P.dma("sp", b2c[:], self.w[pre + "b2"][l].rearrange("(h o) -> h o", o=1), writes=[b2c])
            P.dma("sp", b2r[:], self.w[pre + "b2"][l].partition_broadcast(127), writes=[b2r])
            pc = self.PS[4]
            for j in range(32):
                P.op("pe", lambda e, j=j: e.matmul(pc[:, 0:1], lhsT=w1[:, j, :], rhs=posT[:, j:j + 1], start=(j == 0), stop=(j == 31)), [w1, posT], [pc])
            P.op("dve", lambda e: e.tensor_tensor(out=cb[:, 0:1], in0=pc[:, 0:1], in1=b1[:], op=ALU.add), [pc, b1], [cb])
            for g in range(2):
                ph = self.PS[5]
                for j in range(32):
                    P.op("pe", lambda e, j=j, g=g: e.matmul(ph[:, 0:127], lhsT=w1[:, j, :], rhs=kvT[:, g, j:j + 16 * 126 + 1:16], start=(j == 0), stop=(j == 31)), [w1, kvT], [ph])
                P.op("act", lambda e: e.activation(out=xs_[:, 0:127], in_=ph[:, 0:127], func=AF.Identity, bias=cb[:, 0:1]), [ph, cb], [xs_])
                P.op("dve", lambda e: e.tensor_tensor(out=x2[:, 0:127], in0=xs_[:, 0:127], in1=xs_[:, 0:127], op=ALU.mult), [xs_], [x2])
                P.op("dve", lambda e: e.tensor_scalar(out=x2[:, 0:127], in0=x2[:, 0:127], scalar1=0.044715, scalar2=1.0, op0=ALU.mult, op1=ALU.add), [x2], [x2])
                P.op("dve", lambda e: e.tensor_tensor(out=x2[:, 0:127], in0=x2[:, 0:127], in1=xs_[:, 0:127], op=ALU.mult), [x2, xs_], [x2])
                P.op("act", lambda e: e.activation(out=th[:, 0:127], in_=x2[:, 0:127], func=AF.Tanh, scale=0.7978845608028654), [x2], [th])
                P.op("dve", lambda e: e.tensor_scalar(out=th[:, 0:127], in0=th[:, 0:127], scalar1=1.0, scalar2=0.5, op0=ALU.add, op1=ALU.mult), [th], [th])
                P.op("dve", lambda e: e.tensor_tensor(out=gl[:, 0:127], in0=th[:, 0:127], in1=xs_[:, 0:127], op=ALU.mult), [th, xs_], [gl])
                po = self.PS[6]
                if kind == "k":
                    P.op("pe", lambda e: e.matmul(po[0:64, 0:127], lhsT=w2[:], rhs=gl[:, 0:127], start=True, stop=True), [w2, gl], [po])
                    P.op("act", lambda e, g=g: e.activation(out=kcc[:, g, 0:127], in_=po[0:64, 0:127], func=AF.Identity, bias=b2c[:, 0:1]), [po, b2c], [kcc])
                else:
                    P.op("pe", lambda e: e.matmul(po[0:127, 0:64], lhsT=gl[:, 0:127], rhs=w2[:], start=True, stop=True), [w2, gl], [po])
                    P.op("dve", lambda e, g=g: e.tensor_tensor(out=vca[:, g, 0:64], in0=po[0:127, 0:64], in1=b2r[:], op=ALU.add), [po, b2r], [vca])
        if os.environ.get('KNSTOP') == '1':
            return
        P.barrier()
        ar.off = mark
        ks = ar.alloc(96, [2, S], BF16, "ks")
        kw = ar.alloc(64, [2, S], BF16, "kw")
        vs = ar.alloc(128, [TT, 2, 65], BF16, "vs")
        vw = ar.alloc(128, [TT, 2, 65], BF16, "vw")
        PT = [ar.alloc(128, [512], BF16, "PT") for _ in range(4)]
        ocn = ar.alloc(128, [4, 97], F32, "ocn")
        rdn = [ar.alloc(128, [4], F32, "rdn") for _ in range(4)]
        imp = ar.alloc(128, [32], F32, "imp")
        scr1 = ar.alloc(128, [32], F32, "scr1")
        scr2 = ar.alloc(128, [32], F32, "scr2")
        m8 = ar.alloc(128, [16], F32, "m8")
        nbw = ar.alloc(128, [96], BF16, "nbw")
        tmp = [ar.alloc(128, [4, 64], F32, "tmp") for _ in range(4)]
        P.dma("sp", ks[0:64, :, :], self.scr["ksT"].rearrange("g d t -> d g t"), reads=[self.scrT["ksT"]], writes=[ks])
        for g in range(2):
            P.dma("sp", ks[64:96, g, :], self.cin["c_E"], writes=[ks])
        P.dma("sp", kw[:], self.scr["kwT"].rearrange("g d t -> d g t"), reads=[self.scrT["kwT"]], writes=[kw])
        P.op("pool", lambda e: e.memset(vs[:], 1.0), [], [vs])
        P.op("pool", lambda e: e.memset(vw[:], 1.0), [], [vw])
        P.op("pool", lambda e: e.memset(nbw[:], 0.0), [], [nbw])
        for i in range(TT):
            P.dma("sp", vs[:, i, :, 0:64], self.scr["vs"][i * 128:(i + 1) * 128, :].rearrange("p (h d) -> p h d", d=64), reads=[self.scrT["vs"]], writes=[vs])
            P.dma("sp", vw[:, i, :, 0:64], self.scr["vw"][i * 128:(i + 1) * 128, :].rearrange("p (h d) -> p h d", d=64), reads=[self.scrT["vw"]], writes=[vw])
        kk = 0
        for g in range(2):
            for c in range(4):
                for r in range(4):
                    h = g * 4 + r
                    ps = self.PS[r % 2]
                    pt = PT[r]
                    P.op("pe", lambda e, ps=ps, c=c: e.matmul(ps[0:127, :], lhsT=self.identbig[0:127, 0:127], rhs=self.cmask[:, c * 512:(c + 1) * 512], start=True, stop=False), [self.identbig, self.cmask], [ps])
                    P.op("pe", lambda e, ps=ps, c=c, g=g, h=h: e.matmul(ps[0:127, :], lhsT=kcc[:, g, 0:127], rhs=qn[0:64, h, c * 512:(c + 1) * 512], start=False, stop=True), [kcc, qn], [ps])
                    P.op("act", lambda e, ps=ps, pt=pt: e.activation(out=pt[0:127, :], in_=ps[0:127, :], func=AF.Exp, scale=SCALE), [ps], [pt])
                for il in range(4):
                    i = 4 * c + il
                    oc = self.PS[2 + (kk % 2)]
                    rd = rdn[kk % 2]
                    kk += 1
                    for r in range(4):
                        P.op("pe", lambda e, oc=oc, r=r, il=il, g=g: e.matmul(oc[:, r * 97:(r + 1) * 97], lhsT=PT[r][0:127, il * 128:(il + 1) * 128], rhs=vca[:, g, :], start=True, stop=True), [PT[r], vca], [oc])
                    ov = oc[:, 0:388].rearrange("p (r c) -> p r c", c=97)
                    P.op("dve", lambda e, rd=rd, ov=ov: e.tensor_scalar(out=rd[:], in0=ov[:, :, 64], scalar1=1e-30, scalar2=None, op0=ALU.max), [oc], [rd])
                    P.op("dve", lambda e, rd=rd: e.reciprocal(out=rd[:], in_=rd[:]), [rd], [rd])
                    P.op("dve", lambda e, rd=rd, ov=ov: e.tensor_tensor(out=ocn[:], in0=ov, in1=rd[:].unsqueeze(2).to_broadcast([128, 4, 97]), op=ALU.mult), [oc, rd], [ocn])
                    P.op("dve", lambda e, i=i, g=g: e.tensor_tensor(out=onsa[:, i, g * 256:(g + 1) * 256].rearrange("p (r d) -> p r d", d=64), in0=ocn[:, :, 0:64], in1=g24[:, i, g * 4:g * 4 + 4].unsqueeze(2).to_broadcast([128, 4, 64]), op=ALU.mult), [ocn, g24], [onsa])
                    P.op("dve", lambda e: e.tensor_reduce(out=imp[:], in_=ocn[:, :, 65:97].rearrange("p r j -> p j r"), axis=AX.X, op=ALU.add), [ocn], [imp])
                    P.op("dve", lambda e, i=i: e.tensor_tensor(out=scr1[:], in0=imp[:], in1=self.mimp[:, i, :], op=ALU.mult), [imp, self.mimp], [scr1])
                    P.op("dve", lambda e, i=i: e.tensor_tensor(out=scr1[:], in0=scr1[:], in1=self.fbias[:, i, :], op=ALU.add), [scr1, self.fbias], [scr1])
                    P.op("dve", lambda e: e.max(out=m8[:, 0:8], in_=scr1[:]), [scr1], [m8])
                    P.op("dve", lambda e: e.match_replace(out=scr2[:], in_to_replace=m8[:, 0:8], in_values=scr1[:], imm_value=NEG), [m8, scr1], [scr2])
                    P.op("dve", lambda e: e.max(out=m8[:, 8:16], in_=scr2[:]), [scr2], [m8])
                    P.op("dve", lambda e: e.tensor_scalar(out=nbw[:, 64:96], in0=scr1[:], scalar1=m8[:, 15:16], scalar2=-1.0, op0=ALU.is_ge, op1=ALU.add), [scr1, m8], [nbw])
                    pt2 = self.PS[4 + (kk % 2)]
                    ptv = pt2.h.bitcast(BF16)
                    P.op("pe", lambda e, ptv=ptv: e.transpose(ptv[0:96, 0:128], nbw[:], self.ident[:]), [nbw, self.ident], [pt2])
                    P.op("act", lambda e, ptv=ptv, g=g, i=i: e.copy(out=qn[64:96, g * 4:(g + 1) * 4, i * 128:(i + 1) * 128], in_=ptv[64:96, 0:128].unsqueeze(1).to_broadcast([32, 4, 128])), [pt2], [qn])
        if os.environ.get('KNSTOP') == '2':
            return
        def nsa_head(br, h, slot):
            kt_, vt_, kd = [(ks, vs, 96), (kw, vw, 64)][br]
            g = h // 4
            cnt_ = [0]

            def out_cb(c, osb, h=h, br=br):
                ov = osb[:, 0:260].rearrange("p (a b) -> p a b", b=65)
                rd = rdn[slot * 2 + cnt_[0] % 2]
                tp = tmp[slot * 2 + cnt_[0] % 2]
                cnt_[0] += 1
                P.op("dve", lambda e, rd=rd, ov=ov: e.reciprocal(out=rd[:], in_=ov[:, :, 64]), [osb], [rd])
                P.op("dve", lambda e, rd=rd, c=c, h=h, br=br: e.tensor_tensor(out=rd[:], in0=rd[:], in1=g24[:, 4 * c:4 * c + 4, (br + 1) * 8 + h], op=ALU.mult), [rd, g24], [rd])
                P.op("dve", lambda e, rd=rd, ov=ov, tp=tp: e.tensor_tensor(out=tp[:], in0=ov[:, :, 0:64], in1=rd[:].unsqueeze(2).to_broadcast([128, 4, 64]), op=ALU.mult), [osb, rd], [tp])
                P.op("pool", lambda e, tp=tp, c=c, h=h: e.tensor_tensor(out=onsa[:, 4 * c:4 * c + 4, h * 64:(h + 1) * 64], in0=onsa[:, 4 * c:4 * c + 4, h * 64:(h + 1) * 64], in1=tp[:], op=ALU.add), [tp, onsa], [onsa])

            if br == 0:
                pairs = lambda i: list(range(0, i + 1))
            else:
                pairs = lambda i: list(range(max(0, i - 4), i + 1))

            def nbias(c, j, valid):
                n = len(valid) * 128
                if valid[0] == j:
                    return (self.tz[:, 0:n], self.tz)
                if valid[-1] == j + 4:
                    return (self.za[:, 512 - n:512], self.za)
                return None

            return self.attn_head(lambda j, kt_=kt_, g=g, kd=kd: kt_[0:kd, g, j * 128:(j + 1) * 128],
                                  lambda i0_, i1_, h=h, kd=kd: qn[0:kd, h, i0_ * 128:i1_ * 128],
                                  lambda j, vt_=vt_, g=g: vt_[:, j, g, :], [qn, kt_, vt_],
                                  pairs, nbias, out_cb, PTn[3 * slot:3 * slot + 3], slot=slot)

        PTn = PT + [ar.alloc(128, [512], BF16, "PTx") for _ in range(2)]
        for br in range(2):
            if br == 1 and os.environ.get('KNSTOP') == '3':
                return
            for hp in range(4):
                self.interleave([nsa_head(br, 2 * hp, 0), nsa_head(br, 2 * hp + 1, 1)])
        onb = ar.alloc(128, [TT, 512], BF16, "onb")
        P.op("act", lambda e: e.copy(out=onb[:], in_=onsa[:]), [onsa], [onb])
        self.to_featmajor(onb, self.onT)
        if self.dbg:
            self.dscr_once("dbg_onsa", [S, 512], BF16)
            P.dma("sp", self.scr["dbg_onsa"].rearrange("(i p) c -> p i c", p=128), onb[:], reads=[onb], writes=[self.scrT["dbg_onsa"]])

    def attn_head(self, KT, QTr, V, rds, pairs, bias_fn, out_cb, PT, slot=0):
        P = self.P
        sbanks = [self.PS[0], self.PS[1]] if slot == 0 else [self.PS[6], self.PS[7]]
        obanks = [self.PS[2], self.PS[3]] if slot == 0 else [self.PS[4], self.PS[5]]
        scnt = 0
        ocnt = 0
        for c in range(4):
            tiles = list(range(4 * c, 4 * c + 4))
            js = sorted(set(j for i in tiles for j in pairs(i)))
            osb = obanks[ocnt % 2]
            ocnt += 1
            state = {}

            def emit_S(j, c=c, tiles=tiles):
                nonlocal scnt
                valid = [i for i in tiles if j in pairs(i)]
                ps = sbanks[scnt % 2]
                pt = PT[scnt % len(PT)]
                scnt += 1
                lo = (valid[0] - 4 * c) * 128
                hi = (valid[-1] - 4 * c + 1) * 128
                bz = bias_fn(c, j, valid)
                if bz is not None:
                    bap, bt = bz
                    P.op("pe", lambda e, ps=ps, lo=lo, hi=hi, bap=bap: e.matmul(ps[:, lo:hi], lhsT=self.identbig[:], rhs=bap, start=True, stop=False), [self.identbig, bt], [ps])
                P.op("pe", lambda e, ps=ps, lo=lo, hi=hi, kt_=KT(j), qt_=QTr(valid[0], valid[-1] + 1), nb_=(bz is None): e.matmul(ps[:, lo:hi], lhsT=kt_, rhs=qt_, start=nb_, stop=True), rds, [ps])
                P.op("act", lambda e, ps=ps, pt=pt, lo=lo, hi=hi: e.activation(out=pt[:, lo:hi], in_=ps[:, lo:hi], func=AF.Exp, scale=SCALE), [ps], [pt])
                state[j] = (valid, pt)

            def emit_PV(j, c=c, js=js, osb=osb):
                valid, pt = state[j]
                for i in valid:
                    c0 = (i - 4 * c) * 128
                    o0 = (i - 4 * c) * 65
                    sp_ = (j == js[-1] and i == valid[-1])
                    P.op("pe", lambda e, pt=pt, c0=c0, o0=o0, osb=osb, v_=V(j), sp_=sp_: e.matmul(osb[:, o0:o0 + 65], lhsT=pt[:, c0:c0 + 128], rhs=v_, start=False, stop=sp_), [pt] + rds, [osb])

            P.op("pe", lambda e, osb=osb: e.matmul(osb[:, 0:260], lhsT=self.zeros[:, 0:128], rhs=self.zeros[:, 0:260], start=True, stop=False), [self.zeros], [osb])
            emit_S(js[0])
            yield
            for idx, j in enumerate(js):
                if idx + 1 < len(js):
                    emit_S(js[idx + 1])
                emit_PV(j)
                yield
            out_cb(c, osb)
            yield

    @staticmethod
    def interleave(gens):
        gens = list(gens)
        while gens:
            for g in list(gens):
                try:
                    next(g)
                except StopIteration:
                    gens.remove(g)

    def phase_dsa(self, l):
        P = self.P
        ar = self.ar
        ar.off = 0
        if "mbT" not in self.scr:
            self.dscr("mbT", [136, 128, 128], BF16)
            self.c_triT = P.sb([128, 128], F32, "triT")
            P.dma("sp", self.c_triT[:], self.cin["c_triT"], writes=[self.c_triT])
        mbT_d = self.scr["mbT"]
        mbT_T = self.scrT["mbT"]
        mbT = ar.alloc(128, [136, 128], BF16, "mbT")
        mbT_end = ar.off
        ar.off = 0
        iqT = ar.alloc(64, [8, S], BF16, "iqT")
        ikT = ar.alloc(64, [S], BF16, "ikT")
        assert ar.off >= mbT_end
        iwt = ar.alloc(128, [TT, 8], F32, "iwt")
        dg = [ar.alloc(128, [8, 128], BF16, "dg") for _ in range(2)]
        rb = [ar.alloc(128, [512], BF16, "rb") for _ in range(4)]
        sc = [ar.alloc(128, [128 * (i + 1)], F32, "sc") for i in range(TT)]
        thrc = ar.alloc(128, [TT], F32, "thrc")
        nmid = ar.alloc(128, [TT], F32, "nmid")
        cnt2 = ar.alloc(128, [TT], F32, "cnt2")
        lo = ar.alloc(128, [TT], F32, "lo")
        wv = ar.alloc(128, [TT], F32, "wv")
        mid = ar.alloc(128, [TT], F32, "mid")
        cnt = ar.alloc(128, [TT], F32, "cnt")
        ge = ar.alloc(128, [TT], F32, "ge")
        m8 = ar.alloc(128, [TT, 8], F32, "m8")
        mb = [ar.alloc(128, [S], BF16, "mb") for _ in range(2)]
        P.dma("sp", iqT[:], self.scr["iqT"].rearrange("h d t -> d h t"), reads=[self.scrT["iqT"]], writes=[iqT])
        P.dma("sp", ikT[:], self.scr["ikT"][0], reads=[self.scrT["ikT"]], writes=[ikT])
        self.dma_tok(iwt, self.scr["iw"], reads=[self.scrT["iw"]])
        ka = 0
        kb = 0
        kr = 0
        for i in range(TT):
            d = dg[i % 2]
            identb = self.ident[:].unsqueeze(1).to_broadcast([128, 8, 128])
            iwb = iwt[:, i, :].unsqueeze(2).to_broadcast([128, 8, 128])
            P.op("dve", lambda e, d=d, identb=identb, iwb=iwb: e.tensor_tensor(out=d[:], in0=identb, in1=iwb, op=ALU.mult), [self.ident, iwt], [d])
            L = 128 * (i + 1)
            units = [(sci, h) for sci in range(i // 4 + 1) for h in range(8)]
            pAb = [self.PS[0], self.PS[1], self.PS[6], self.PS[7]]
            st_ = {}

            def emit_front(u, i=i, L=L):
                nonlocal ka, kr
                sci, h = units[u]
                ncol = min(512, L - 512 * sci)
                pA = pAb[ka % 4]
                ka += 1
                r = rb[kr % 4]
                kr += 1
                P.op("pe", lambda e, pA=pA, h=h, i=i, sci=sci, ncol=ncol: e.matmul(pA[:, 0:ncol], lhsT=iqT[:, h, i * 128:(i + 1) * 128], rhs=ikT[:, sci * 512:sci * 512 + ncol], start=True, stop=True), [iqT, ikT], [pA])
                if h % 2 == 0:
                    P.op("act", lambda e, pA=pA, r=r, ncol=ncol: e.activation(out=r[:, 0:ncol], in_=pA[:, 0:ncol], func=AF.Relu), [pA], [r])
                else:
                    P.op("dve", lambda e, pA=pA, r=r, ncol=ncol: e.tensor_scalar(out=r[:, 0:ncol], in0=pA[:, 0:ncol], scalar1=0.0, scalar2=None, op0=ALU.max), [pA], [r])
                st_[u] = (r, ncol)

            def emit_back(u, i=i, d=d):
                nonlocal kb
                sci, h = units[u]
                r, ncol = st_[u]
                pB = self.PS[2 + (kb % 2)]
                P.op("pe", lambda e, pB=pB, d=d, h=h, r=r, ncol=ncol: e.matmul(pB[:, 0:ncol], lhsT=d[:, h, :], rhs=r[:, 0:ncol], start=(h == 0), stop=(h == 7)), [d, r], [pB])
                if h == 7:
                    P.op("act", lambda e, pB=pB, i=i, sci=sci, ncol=ncol: e.copy(out=sc[i][:, sci * 512:sci * 512 + ncol], in_=pB[:, 0:ncol]), [pB], [sc[i]])
                    kb += 1

            emit_front(0)
            emit_front(1)
            for u in range(len(units)):
                if u + 2 < len(units):
                    emit_front(u + 2)
                emit_back(u)
            P.op("pool", lambda e, i=i: e.tensor_tensor(out=sc[i][:, i * 128:(i + 1) * 128], in0=sc[i][:, i * 128:(i + 1) * 128], in1=self.c_triT[:], op=ALU.add), [sc[i], self.c_triT], [sc[i]])
        if os.environ.get('KDSTOP') == '1':
            return
        P.op("dve", lambda e: e.memset(lo[:], -1e29), [], [lo])
        for i in range(2, TT):
            P.op("dve", lambda e, i=i: e.max(out=m8[:, i, :], in_=sc[i][:]), [sc[i]], [m8])
            P.op("dve", lambda e, i=i: e.tensor_reduce(out=lo[:, i:i + 1], in_=sc[i][:, 0:128 * i], axis=AX.X, op=ALU.min), [sc[i]], [lo])
        P.op("dve", lambda e: e.tensor_tensor(out=wv[:, 2:TT], in0=m8[:, 2:TT, 0], in1=lo[:, 2:TT], op=ALU.subtract), [m8, lo], [wv])
        ACT_TILES = (10, 11, 12, 13, 14, 15)
        P.op("dve", lambda e: e.memset(thrc[:], 255.5), [], [thrc])
        for i in ACT_TILES:
            P.op("dve", lambda e, i=i: e.memset(thrc[:, i:i + 1], 511.0 - 128.0 * (i + 1)), [], [thrc])
        for it in range(N_ITERS):
            P.op("dve", lambda e: e.tensor_scalar(out=wv[:, 2:TT], in0=wv[:, 2:TT], scalar1=0.5, scalar2=None, op0=ALU.mult), [wv], [wv])
            P.op("dve", lambda e: e.tensor_tensor(out=mid[:, 2:TT], in0=lo[:, 2:TT], in1=wv[:, 2:TT], op=ALU.add), [lo, wv], [mid])
            P.op("dve", lambda e: e.tensor_scalar(out=nmid[:, 2:TT], in0=mid[:, 2:TT], scalar1=-1.0, scalar2=None, op0=ALU.mult), [mid], [nmid])
            for i in range(TT - 1, 1, -1):
                if i in ACT_TILES:
                    P.op("act", lambda e, i=i: e.activation(out=mb[1][:, 0:128 * (i + 1)], in_=sc[i][:], func=AF.Sign, bias=nmid[:, i:i + 1], scale=1.0, accum_out=cnt[:, i:i + 1]), [sc[i], nmid], [mb[1], cnt])
            for i in range(2, TT):
                if i not in ACT_TILES:
                    P.op("dve", lambda e, i=i: e.tensor_scalar(out=mb[0][:, 0:128 * (i + 1)], in0=sc[i][:], scalar1=mid[:, i:i + 1], scalar2=None, op0=ALU.is_ge, op1=ALU.add, accum_out=cnt2[:, i:i + 1]), [sc[i], mid], [mb[0], cnt2])
            for i in range(2, TT):
                pass
            P.op("dve", lambda e: e.tensor_copy(out=cnt2[:, 10:16], in_=cnt[:, 10:16]), [cnt], [cnt2])
            P.op("dve", lambda e: e.tensor_tensor(out=ge[:, 2:TT], in0=cnt2[:, 2:TT], in1=thrc[:, 2:TT], op=ALU.is_ge), [cnt2, thrc], [ge])
            P.op("dve", lambda e: e.tensor_tensor(out=ge[:, 2:TT], in0=ge[:, 2:TT], in1=wv[:, 2:TT], op=ALU.mult), [ge, wv], [ge])
            P.op("dve", lambda e: e.tensor_tensor(out=lo[:, 2:TT], in0=lo[:, 2:TT], in1=ge[:, 2:TT], op=ALU.add), [lo, ge], [lo])
        if os.environ.get('KDSTOP') == '2':
            return
        P.barrier()
        kt = 0
        for i in range(TT):
            m = mb[i % 2]
            L = 128 * (i + 1)
            P.op("dve", lambda e, m=m, i=i, L=L: e.tensor_scalar(out=m[:, 0:L], in0=sc[i][:], scalar1=lo[:, i:i + 1], scalar2=-1.0, op0=ALU.is_ge, op1=ALU.add), [sc[i], lo], [m])
            for j0 in range(0, i + 1, 4):
                nbk = min(4, i + 1 - j0)
                pt = self.PS[4 + (kt % 2)]
                kt += 1
                ptv = pt.h.bitcast(BF16)
                for jj in range(nbk):
                    j = j0 + jj
                    P.op("pe", lambda e, ptv=ptv, jj=jj, m=m, j=j: e.transpose(ptv[:, jj * 128:(jj + 1) * 128], m[:, j * 128:(j + 1) * 128], self.ident[:]), [m, self.ident], [pt])
                for jj in range(nbk):
                    j = j0 + jj
                    bi_ = 16 * j - j * (j - 1) // 2 + (i - j)
                    if jj % 2 == 0:
                        P.op("act", lambda e, ptv=ptv, jj=jj, bi_=bi_: e.copy(out=mbT[:, bi_, :], in_=ptv[:, jj * 128:(jj + 1) * 128]), [pt], [mbT])
                    else:
                        P.op("dve", lambda e, ptv=ptv, jj=jj, bi_=bi_: e.tensor_copy(out=mbT[:, bi_, :], in_=ptv[:, jj * 128:(jj + 1) * 128]), [pt], [mbT])
        if self.dbg:
            self.dscr_once("dbg_thr", [128, TT], F32)
            P.dma("sp", self.scr["dbg_thr"], lo[:], reads=[lo], writes=[self.scrT["dbg_thr"]])
        if os.environ.get('KDSTOP') == '3':
            return
        P.barrier()
        ar.off = mbT_end
        vd = ar.alloc(128, [TT, 8, 65], BF16, "vdaug")
        qh = [ar.alloc(64, [S], BF16, "qh") for _ in range(4)]
        kh = [ar.alloc(64, [S], BF16, "kh") for _ in range(4)]
        PT = [ar.alloc(128, [512], BF16, "PT") for _ in range(6)]
        odsa = ar.alloc(128, [TT, 512], BF16, "odsa")
        rden = [ar.alloc(128, [4], F32, "rden") for _ in range(4)]
        P.op("pool", lambda e: e.memset(vd[:], 1.0), [], [vd])
        for i in range(TT):
            P.dma("sp", vd[:, i, :, 0:64], self.scr["vd"][i * 128:(i + 1) * 128, :].rearrange("p (h d) -> p h d", d=64), reads=[self.scrT["vd"]], writes=[vd])
        def dsa_head(h, slot):
            q = qh[h % 4]
            k_ = kh[h % 4]
            P.dma("sp", q[:], self.scr["qdT"][h], reads=[self.scrT["qdT"]], writes=[q])
            P.dma("sp", k_[:], self.scr["kdT"][h], reads=[self.scrT["kdT"]], writes=[k_])
            cnt_ = [0]

            def out_cb(c, osb, h=h):
                ov = osb[:, 0:260].rearrange("p (a b) -> p a b", b=65)
                rd = rden[slot * 2 + cnt_[0] % 2]
                cnt_[0] += 1
                P.op("dve", lambda e, rd=rd, ov=ov: e.reciprocal(out=rd[:], in_=ov[:, :, 64]), [osb], [rd])
                P.op("dve", lambda e, rd=rd, ov=ov, c=c, h=h: e.tensor_tensor(out=odsa[:, 4 * c:4 * c + 4, h * 64:(h + 1) * 64], in0=ov[:, :, 0:64], in1=rd[:].unsqueeze(2).to_broadcast([128, 4, 64]), op=ALU.mult), [osb, rd], [odsa])

            def dbias(c, j, valid):
                b0 = 16 * j - j * (j - 1) // 2 + (valid[0] - j)
                return (mbT[:, b0:b0 + len(valid), :].rearrange("p b t -> p (b t)"), mbT)

            return self.attn_head(lambda j, k_=k_: k_[:, j * 128:(j + 1) * 128], lambda i0_, i1_, q=q: q[:, i0_ * 128:i1_ * 128],
                                  lambda j, h=h: vd[:, j, h, :], [q, k_, vd],
                                  lambda i: list(range(0, i + 1)), dbias, out_cb, PT[3 * slot:3 * slot + 3], slot=slot)

        for hp in range(4):
            self.interleave([dsa_head(2 * hp, 0), dsa_head(2 * hp + 1, 1)])
        self.to_featmajor(odsa, self.odT)
        if self.dbg:
            self.dscr_once("dbg_odsa", [S, 512], BF16)
            P.dma("sp", self.scr["dbg_odsa"].rearrange("(i p) c -> p i c", p=128), odsa[:], reads=[odsa], writes=[self.scrT["dbg_odsa"]])

    def dma_tok(self, dst, src_dram, reads=()):
        v = src_dram.rearrange("(i p) c -> p i c", p=128)
        for q4 in range(4):
            self.P.dma("sp", dst[:, 4 * q4:4 * q4 + 4, :], v[:, 4 * q4:4 * q4 + 4, :], reads=list(reads), writes=[dst])

    def dscr_once(self, name, shape, dtype):
        if name not in self.scr:
            self.dscr(name, shape, dtype)

    def to_featmajor(self, tok, dstT):
        P = self.P
        for i in range(TT):
            pt = self.PS[4 + (i % 2)]
            ptv = pt.h.bitcast(BF16)
            for fc in range(4):
                P.op("pe", lambda e, ptv=ptv, fc=fc, i=i: e.transpose(ptv[:, fc * 128:(fc + 1) * 128], tok[:, i, fc * 128:(fc + 1) * 128], self.ident[:]), [tok, self.ident], [pt])
            P.op("act", lambda e, ptv=ptv, i=i: e.copy(out=dstT[:, :, i * 128:(i + 1) * 128], in_=ptv[:, 0:512].rearrange("p (f t) -> p f t", t=128)), [pt], [dstT])

    def phase_merge(self, b, l, src, srcT):
        P = self.P
        ar = self.ar
        ar.off = 0
        wbn = ar.alloc(128, [4, D], BF16, "wbn")
        wbd = ar.alloc(128, [4, D], BF16, "wbd")
        wo = ar.alloc(128, [8, D], BF16, "wo")
        wg = [ar.alloc(128, [8, 256], BF16, "wg") for _ in range(2)]
        mT = ar.alloc(128, [8, S], BF16, "mT")
        sg = [ar.alloc(128, [512], F32, "sg") for _ in range(8)]
        xt = [ar.alloc(128, [D], F32, "xt") for _ in range(2)]
        P.dma("pool", wbn[:], self.w["w_branch_nsa"][l].rearrange("(kc p) c -> p kc c", p=128), writes=[wbn])
        P.dma("pool", wbd[:], self.w["w_branch_dsa"][l].rearrange("(kc p) c -> p kc c", p=128), writes=[wbd])
        P.dma("pool", wo[:], self.w["w_out"][l].rearrange("(kc p) c -> p kc c", p=128), writes=[wo])
        win = self.w["w_in"][l]
        k = 0
        for cc in range(8):
            g = wg[cc % 2]
            P.dma("pool", g[:, :, 0:128], win[:, 3424 + cc * 128:3424 + (cc + 1) * 128].rearrange("(kc p) c -> p kc c", p=128), writes=[g])
            P.dma("pool", g[:, :, 128:256], win[:, 4448 + cc * 128:4448 + (cc + 1) * 128].rearrange("(kc p) c -> p kc c", p=128), writes=[g])
            for tc in range(4):
                bs_ = 4 * ((cc * 4 + tc) % 2)
                pyn, pyd, pgn, pgd = self.PS[bs_], self.PS[bs_ + 1], self.PS[bs_ + 2], self.PS[bs_ + 3]
                ts_ = slice(tc * 512, (tc + 1) * 512)
                for fc in range(4):
                    P.op("pe", lambda e, fc=fc, cc=cc, ts_=ts_, pyn=pyn: e.matmul(pyn[:], lhsT=wbn[:, fc, cc * 128:(cc + 1) * 128], rhs=self.onT[:, fc, ts_], start=(fc == 0), stop=(fc == 3)), [wbn, self.onT], [pyn])
                for fc in range(4):
                    P.op("pe", lambda e, fc=fc, cc=cc, ts_=ts_, pyd=pyd: e.matmul(pyd[:], lhsT=wbd[:, fc, cc * 128:(cc + 1) * 128], rhs=self.odT[:, fc, ts_], start=(fc == 0), stop=(fc == 3)), [wbd, self.odT], [pyd])
                ht = self.hT[tc]
                for kc in range(8):
                    P.op("pe", lambda e, kc=kc, g=g, ht=ht, pgn=pgn: e.matmul(pgn[:], lhsT=g[:, kc, 0:128], rhs=ht[:, kc, :], start=(kc == 0), stop=(kc == 7)), [g, ht], [pgn])
                for kc in range(8):
                    P.op("pe", lambda e, kc=kc, g=g, ht=ht, pgd=pgd: e.matmul(pgd[:], lhsT=g[:, kc, 128:256], rhs=ht[:, kc, :], start=(kc == 0), stop=(kc == 7)), [g, ht], [pgd])
                s1, s2, m1, m2 = sg[bs_:bs_ + 4]
                P.op("act", lambda e, s1=s1, pgn=pgn: e.activation(out=s1[:], in_=pgn[:], func=AF.Sigmoid), [pgn], [s1])
                P.op("act", lambda e, s2=s2, pgd=pgd: e.activation(out=s2[:], in_=pgd[:], func=AF.Sigmoid), [pgd], [s2])
                P.op("dve", lambda e, m1=m1, s1=s1, pyn=pyn: e.tensor_tensor(out=m1[:], in0=pyn[:], in1=s1[:], op=ALU.mult), [pyn, s1], [m1])
                P.op("dve", lambda e, m2=m2, s2=s2, pyd=pyd: e.tensor_tensor(out=m2[:], in0=pyd[:], in1=s2[:], op=ALU.mult), [pyd, s2], [m2])
                P.op("pool", lambda e, cc=cc, ts_=ts_, m1=m1, m2=m2: e.tensor_tensor(out=mT[:, cc, ts_], in0=m1[:], in1=m2[:], op=ALU.add), [m1, m2], [mT])
        for i in range(TT):
            x = xt[i % 2]
            rd = [srcT[i]] if srcT is not None else []
            P.dma("sp", x[:], src[i * 128:(i + 1) * 128, :], reads=rd, writes=[x])
            for half in range(2):
                ps = self.PS[4 + (k % 2)]
                k += 1
                for cc in range(8):
                    P.op("pe", lambda e, ps=ps, cc=cc, i=i, half=half: e.matmul(ps[:], lhsT=mT[:, cc, i * 128:(i + 1) * 128], rhs=wo[:, cc, half * 512:(half + 1) * 512], start=(cc == 0), stop=(cc == 7)), [mT, wo], [ps])
                P.op("dve", lambda e, ps=ps, x=x, half=half: e.tensor_tensor(out=x[:, half * 512:(half + 1) * 512], in0=ps[:], in1=x[:, half * 512:(half + 1) * 512], op=ALU.add), [ps, x], [x])
            P.dma("sp", self.scr["xs"][i * 128:(i + 1) * 128, :], x[:], reads=[x], writes=[self.xsT[i]])

    def phase_ffn(self, b, l):
        P = self.P
        ar = self.ar
        P.barrier()
        ar.off = 0
        actT = ar.alloc(128, [NFC, S], BF16, "actT")
        wgu = [ar.alloc(128, [8, 256], BF16, "wgu") for _ in range(2)]
        wd = ar.alloc(128, [NFC, 512], BF16, "wd")
        sgb = [ar.alloc(128, [512], F32, "sgb") for _ in range(2)]
        xt = [ar.alloc(128, [512], F32, "xh") for _ in range(2)]
        wgate = self.w["w_ffn_gate"][l]
        wup = self.w["w_ffn_up"][l]
        wdn = self.w["w_ffn_down"][l]
        k = 0
        for fc in range(NFC):
            g = wgu[fc % 2]
            P.dma("pool", g[:, :, 0:128], wgate[:, fc * 128:(fc + 1) * 128].rearrange("(kc p) c -> p kc c", p=128), writes=[g])
            P.dma("pool", g[:, :, 128:256], wup[:, fc * 128:(fc + 1) * 128].rearrange("(kc p) c -> p kc c", p=128), writes=[g])
            for tc in range(4):
                pg = self.PS[(k % 2) * 2]
                pu = self.PS[(k % 2) * 2 + 1]
                sb_ = sgb[k % 2]
                k += 1
                ht = self.hT[tc]
                for kc in range(8):
                    P.op("pe", lambda e, kc=kc, g=g, ht=ht, pg=pg: e.matmul(pg[:], lhsT=g[:, kc, 0:128], rhs=ht[:, kc, :], start=(kc == 0), stop=(kc == 7)), [g, ht], [pg])
                for kc in range(8):
                    P.op("pe", lambda e, kc=kc, g=g, ht=ht, pu=pu: e.matmul(pu[:], lhsT=g[:, kc, 128:256], rhs=ht[:, kc, :], start=(kc == 0), stop=(kc == 7)), [g, ht], [pu])
                P.op("act", lambda e, sb_=sb_, pg=pg: e.activation(out=sb_[:], in_=pg[:], func=AF.Silu), [pg], [sb_])
                P.op("dve", lambda e, sb_=sb_, pu=pu, fc=fc, tc=tc: e.tensor_tensor(out=actT[:, fc, tc * 512:(tc + 1) * 512], in0=pu[:], in1=sb_[:], op=ALU.mult), [pu, sb_], [actT])
        for half in range(2):
            P.dma("pool", wd[:], wdn[:, half * 512:(half + 1) * 512].rearrange("(kc p) c -> p kc c", p=128), writes=[wd])
            for i in range(TT):
                x = xt[i % 2]
                P.dma("sp", x[:], self.scr["xs"][i * 128:(i + 1) * 128, half * 512:(half + 1) * 512], reads=[self.xsT[i]], writes=[x])
                ps = self.PS[4 + (i % 2)]
                for fc in range(NFC):
                    P.op("pe", lambda e, ps=ps, fc=fc, i=i: e.matmul(ps[:], lhsT=actT[:, fc, i * 128:(i + 1) * 128], rhs=wd[:, fc, :], start=(fc == 0), stop=(fc == NFC - 1)), [actT, wd], [ps])
                P.op("dve", lambda e, ps=ps, x=x: e.tensor_tensor(out=x[:], in0=ps[:], in1=x[:], op=ALU.add), [ps, x], [x])
                P.dma("sp", self.scr["xs"][i * 128:(i + 1) * 128, half * 512:(half + 1) * 512], x[:], reads=[x], writes=[self.xsT[i]])

    def phase_final(self, b):
        P = self.P
        ar = self.ar
        ar.off = 0
        P.dma("sp", self.gvec[:], self.w["final_norm"].partition_broadcast(128), writes=[self.gvec])
        xt = [ar.alloc(128, [D], F32, "xf") for _ in range(3)]
        yt = [ar.alloc(128, [D], F32, "yf") for _ in range(2)]
        junk = ar.alloc(128, [D], BF16, "junkf")
        st = [ar.alloc(128, [4], F32, "stf") for _ in range(2)]
        for i in range(TT):
            x = xt[i % 3]
            y = yt[i % 2]
            s = st[i % 2]
            P.dma("sp", x[:], self.scr["xs"][i * 128:(i + 1) * 128, :], reads=[self.xsT[i]], writes=[x])
            P.op("act", lambda e, x=x, s=s: e.activation(out=junk[:], in_=x[:], func=AF.Square, accum_out=s[:, 0:1]), [x], [junk, s])
            P.op("dve", lambda e, s=s: e.tensor_scalar(out=s[:, 1:2], in0=s[:, 0:1], scalar1=1.0 / D, scalar2=EPS, op0=ALU.mult, op1=ALU.add), [s], [s])
            P.op("act", lambda e, s=s: e.activation(out=s[:, 2:3], in_=s[:, 1:2], func=AF.Sqrt), [s], [s])
            P.op("dve", lambda e, s=s: e.reciprocal(out=s[:, 3:4], in_=s[:, 2:3]), [s], [s])
            P.op("dve", lambda e, x=x, s=s, y=y: e.scalar_tensor_tensor(out=y[:], in0=x[:], scalar=s[:, 3:4], in1=self.gvec[:], op0=ALU.mult, op1=ALU.mult), [x, s, self.gvec], [y])
            P.dma("sp", self.y_out[b, i * 128:(i + 1) * 128, :], y[:], reads=[y])


_CACHE = {}


def kernel(**inputs):
    ncores = 8
    nb = 2
    B = Builder(nb, 2)
    nc = B.build()
    consts = host_consts()
    in_maps = []
    for c in range(ncores):
        m = {"x": np.ascontiguousarray(inputs["x"][c * nb:(c + 1) * nb])}
        for k in B.w:
            m[k] = np.ascontiguousarray(inputs[k])
        m.update(consts)
        in_maps.append(m)
    res = run_bass_kernel_spmd(nc, in_maps, core_ids=list(range(ncores)))
    return np.concatenate([np.asarray(r["y"]) for r in res.results], axis=0).astype(np.float32)
```

```python
import os
import numpy as np
from contextlib import ExitStack
import ml_dtypes
import concourse.bass as bass
import concourse.mybir as mybir
from concourse.bass_utils import run_bass_kernel_spmd

F32 = mybir.dt.float32
BF16 = mybir.dt.bfloat16
AF = mybir.ActivationFunctionType
ALU = mybir.AluOpType
AX = mybir.AxisListType

S = 2048
D = 1024
TT = 16
DIN = 5472
DFF = 2816
NFC = 22
EPS = 1e-6
SCALE = 0.125
IDXS = 512.0 ** -0.5
BIG = 30000.0
NEG = -1e30
N_ITERS = 14


class T:
    __slots__ = ("h", "name", "writer", "readers", "dsem", "dcount", "excl", "eph")

    def __init__(self, h, name):
        self.h = h
        self.name = name
        self.writer = None
        self.readers = {}
        self.dsem = None
        self.dcount = 0
        self.excl = False
        self.eph = False

    def __getitem__(self, idx):
        return self.h[idx]


class Eng:
    def __init__(self, name, sem):
        self.name = name
        self.sem = sem
        self.count = 0
        self.known = {}
        self.prog = []


class Prog:
    def __init__(self, nc, stack):
        self.nc = nc
        self.stack = stack
        self.engs = {}
        for n in ("pe", "act", "dve", "pool", "sp"):
            sem = stack.enter_context(nc.semaphore("s_" + n))
            self.engs[n] = Eng(n, sem)
        self.ninstr = 0
        self.uid = 0
        self.dtiles = []

    def sb(self, shape, dtype, name="t"):
        self.uid += 1
        name = "%s_%d" % (name, self.uid)
        h = self.stack.enter_context(self.nc.sbuf_tensor(name, list(shape), dtype))
        return T(h, name)

    def ps(self, shape, dtype=F32, name="p"):
        self.uid += 1
        name = "%s_%d" % (name, self.uid)
        h = self.stack.enter_context(self.nc.psum_tensor(name, list(shape), dtype))
        t = T(h, name)
        t.excl = True
        return t

    def dummy(self, name):
        return T(None, name)

    def _dsem(self, t, q="sp"):
        kind = "sw" if q == "pool" else "hw"
        if t.dsem is None:
            pool = getattr(self, "sem_pool", {}).get(kind)
            if t.eph and pool:
                t.dsem, t.dcount = pool.pop()
            else:
                t.dsem = self.stack.enter_context(self.nc.semaphore("d_" + t.name))
            t.name = t.name + ":" + kind
            self.dtiles.append(t)
        else:
            assert t.name.endswith(":" + kind), "mixed DMA kinds on tile %s" % t.name
        return t.dsem

    def _deps(self, E, reads, writes):
        need = {}
        own = id(E.sem)
        is_pe = E.name == "pe"

        def add(sv, raw):
            if sv is None:
                return
            s, v = sv
            k = id(s)
            if k == own and is_pe:
                return
            if E.known.get(k, 0) >= v:
                return
            if k not in need or need[k][1] < v:
                need[k] = (s, v)

        for t in reads:
            add(t.writer, True)
            if t.excl:
                for sv in t.readers.values():
                    if sv[0] is not E.sem:
                        add(sv, False)
        for t in writes:
            add(t.writer, False)
            for sv in t.readers.values():
                add(sv, False)
        for k, (s, v) in need.items():
            E.known[k] = v
        return list(need.values())

    def op(self, eng, fn, reads=(), writes=()):
        E = self.engs[eng]
        waits = self._deps(E, reads, writes)
        E.count += 1
        sv = (E.sem, E.count)
        for t in reads:
            t.readers[id(E.sem)] = sv
        for t in writes:
            t.writer = sv
            t.readers = {}
        sem = E.sem

        def run(e, waits=waits, fn=fn, sem=sem):
            for (s, v) in waits:
                e.wait_ge(s, v)
            fn(e).then_inc(sem, 1)
        E.prog.append(run)
        self.ninstr += 1

    def dma(self, q, out_ap, in_ap, reads=(), writes=(), **kw):
        E = self.engs[q]
        waits = self._deps(E, reads, writes)
        owner = writes[0] if writes else reads[0]
        sem = self._dsem(owner, q)
        owner.dcount += 16
        sv = (sem, owner.dcount)
        for t in reads:
            t.readers[id(sem)] = sv
        for t in writes:
            t.writer = sv
            t.readers = {}

        def run(e, waits=waits, sem=sem, out_ap=out_ap, in_ap=in_ap, kw=kw):
            for (s, v) in waits:
                e.wait_ge(s, v)
            e.dma_start(out=out_ap, in_=in_ap, **kw).then_inc(sem, 16)
        E.prog.append(run)
        self.ninstr += 1

    def barrier(self):
        front = [(E.sem, E.count) for E in self.engs.values() if E.count > 0]
        front += [(t.dsem, t.dcount) for t in self.dtiles if t.dcount > 0]
        for E in self.engs.values():
            waits = []
            for (s, v) in front:
                if s is E.sem:
                    continue
                if E.known.get(id(s), 0) >= v:
                    continue
                E.known[id(s)] = v
                waits.append((s, v))

            def run(e, waits=waits):
                for (s, v) in waits:
                    e.wait_ge(s, v)
            E.prog.append(run)
        if not hasattr(self, "sem_pool"):
            self.sem_pool = {"sw": [], "hw": []}
        keep = []
        for t in self.dtiles:
            if t.eph:
                self.sem_pool[t.name[-2:]].append((t.dsem, t.dcount))
            else:
                keep.append(t)
        self.dtiles = keep

    def build(self):
        nc = self.nc
        with nc.Block() as block:
            @block.tensor
            def _(e):
                for f in self.engs["pe"].prog:
                    f(e)

            @block.scalar
            def _(e):
                for f in self.engs["act"].prog:
                    f(e)

            @block.vector
            def _(e):
                for f in self.engs["dve"].prog:
                    f(e)

            @block.gpsimd
            def _(e):
                for f in self.engs["pool"].prog:
                    f(e)

            @block.sync
            def _(e):
                for f in self.engs["sp"].prog:
                    f(e)


class Arena:
    def __init__(self, P, nbytes):
        self.P = P
        self.t = P.sb([128, nbytes // 4], F32, "arena")
        self.off = 0
        self.cap = nbytes
        self.n = 0

    def alloc(self, parts, free, dtype, name="a"):
        n = int(np.prod(free))
        esz = 4 if dtype == F32 else 2
        nb = (n * esz + 63) // 64 * 64
        assert self.off + nb <= self.cap, "arena overflow %s %d" % (name, self.off + nb)
        ap = self.t.h[0:parts, self.off // 4:(self.off + nb) // 4]
        if dtype != F32:
            ap = ap.bitcast(dtype)
        ap = ap[:, 0:n]
        if len(free) == 2:
            ap = ap.rearrange("p (a b) -> p a b", a=free[0])
        elif len(free) == 3:
            ap = ap.rearrange("p (a b c) -> p a b c", a=free[0], b=free[1])
        self.off += nb
        self.n += 1
        t = T(ap, "%s%d" % (name, self.n))
        t.eph = True
        return t


def host_consts():
    bf = ml_dtypes.bfloat16
    c = {}
    c["c_ident"] = np.eye(128, dtype=np.float32).astype(bf)
    c["c_identbig"] = (np.eye(128, dtype=np.float32) * BIG).astype(bf)
    pos = np.arange(S, dtype=np.float32)
    inv = (500000.0 ** (-np.arange(0, 16, 2, dtype=np.float32) / 16)).astype(np.float32)
    ang = pos[:, None] * inv[None, :]
    cos = np.cos(ang).astype(np.float32)
    sin = np.sin(ang).astype(np.float32)
    c["c_cos2"] = np.concatenate([cos, cos], 1).astype(np.float32)
    c["c_sin2"] = np.concatenate([-sin, sin], 1).astype(np.float32)
    k = np.arange(128)[:, None]
    q = np.arange(128)[None, :]
    c["c_tri"] = np.where(k <= q, 0.0, -1.0).astype(bf)
    c["c_anti"] = np.where(k > q, 0.0, -1.0).astype(bf)
    c["c_triT"] = np.where(q <= k, 0.0, NEG).astype(np.float32)
    j = np.arange(32)[:, None]
    kk = np.arange(S)[None, :]
    c["c_E"] = (kk // 64 == j).astype(np.float32).astype(bf)
    cc = np.arange(127)[:, None]
    c["c_cmask"] = np.where(16 * cc + 31 <= kk, 0.0, -1.0).astype(bf)
    c_start = np.arange(127)[:, None] * 16
    s_start = np.arange(32)[None, :] * 64
    ov = np.minimum(c_start + 32, s_start + 64) - np.maximum(c_start, s_start)
    c["c_ov"] = (np.clip(ov, 0, None) / 32.0).astype(np.float32).astype(bf)
    t = np.arange(S)[:, None]
    jj = np.arange(32)[None, :]
    cur = t // 64
    dist = cur - jj
    forced = (jj == 0) | ((dist >= 0) & (dist < 2))
    adm = jj * 64 <= t
    c["c_mimp"] = (adm & ~forced).astype(np.float32)
    c["c_fbias"] = np.where(adm, np.where(forced, 1e4, 0.0), NEG).astype(np.float32)
    return c


def proj_chunks():
    ch = []
    ch.append((0, 512, [(i * 128, 128, "T", True, ("qnT", 2 * i)) for i in range(4)]))
    ch.append((512, 512, [(0, 128, "T", True, ("kcT", 0)), (128, 128, "T", False, ("vcT", 0)),
                          (256, 128, "T", True, ("ksT", 0)), (384, 128, "tok", False, ("vs", 0))]))
    ch.append((1024, 280, [(0, 128, "T", True, ("kwT", 0)), (128, 128, "tok", False, ("vw", 0)),
                           (256, 24, "sig", False, ("g24", 0))]))
    ch.append((1304, 512, [(i * 128, 128, "T", True, ("qdT", 2 * i)) for i in range(4)]))
    ch.append((1816, 512, [(i * 128, 128, "T", True, ("kdT", 2 * i)) for i in range(4)]))
    ch.append((2328, 512, [(i * 128, 128, "tok", False, ("vd", 2 * i)) for i in range(4)]))
    ch.append((2840, 512, [(i * 128, 128, "T", True, ("iqT", 2 * i)) for i in range(4)]))
    ch.append((3352, 72, [(0, 64, "T", True, ("ikT", 0)), (64, 8, "scale", False, ("iw", 0))]))
    return ch


class Builder:
    def __init__(self, nb, nl, dbg=False, phases=None):
        self.nb = nb
        self.nl = nl
        self.dbg = dbg
        self.phases = phases
        self.nc = bass.Bass("TRN2", target_bir_lowering=False)
        self.st = ExitStack()

    def din(self, name, shape, dtype=F32):
        return self.nc.dram_tensor(name, list(shape), dtype, kind="ExternalInput").ap()

    def dscr(self, name, shape, dtype):
        kind = "ExternalOutput" if self.dbg else "Internal"
        ap = self.nc.dram_tensor(name, list(shape), dtype, kind=kind).ap()
        self.scr[name] = ap
        self.scrT[name] = self.P.dummy(name)
        return ap

    def build(self):
        nc = self.nc
        nb, nl = self.nb, self.nl
        with self.st:
            P = self.P = Prog(nc, self.st)
            self.scr = {}
            self.scrT = {}
            self.x_in = self.din("x", [nb, S, D])
            self.w = {}
            for name, shape in [("attn_norm", [nl, D]), ("w_in", [nl, D, DIN]),
                                ("cmpk_pos", [nl, 32, 64]), ("cmpk_w1", [nl, 2048, 128]), ("cmpk_b1", [nl, 128]),
                                ("cmpk_w2", [nl, 128, 64]), ("cmpk_b2", [nl, 64]),
                                ("cmpv_pos", [nl, 32, 64]), ("cmpv_w1", [nl, 2048, 128]), ("cmpv_b1", [nl, 128]),
                                ("cmpv_w2", [nl, 128, 64]), ("cmpv_b2", [nl, 64]),
                                ("w_branch_nsa", [nl, 512, D]), ("w_branch_dsa", [nl, 512, D]), ("w_out", [nl, D, D]),
                                ("ffn_norm", [nl, D]), ("w_ffn_gate", [nl, D, DFF]), ("w_ffn_up", [nl, D, DFF]),
                                ("w_ffn_down", [nl, DFF, D]), ("final_norm", [D])]:
                self.w[name] = self.din(name, shape)
            hc = host_consts()
            self.cin = {}
            for k, v in hc.items():
                self.cin[k] = self.din(k, v.shape, BF16 if v.dtype == ml_dtypes.bfloat16 else F32)
            self.y_out = nc.dram_tensor("y", [nb, S, D], F32, kind="ExternalOutput").ap()
            self.dscr("xs", [S, D], F32)
            for nm, nh in [("qnT", 8), ("kcT", 2), ("vcT", 2), ("ksT", 2), ("kwT", 2), ("qdT", 8), ("kdT", 8), ("iqT", 8), ("ikT", 1)]:
                self.dscr(nm, [nh, 64, S], BF16)
            self.dscr("vs", [S, 128], BF16)
            self.dscr("vw", [S, 128], BF16)
            self.dscr("vd", [S, 512], BF16)
            self.dscr("g24", [S, 24], F32)
            self.dscr("iw", [S, 8], F32)
            self.xsT = [P.dummy("xs%d" % i) for i in range(TT)]
            self.PS = [P.ps([128, 512], F32, "bank") for _ in range(8)]
            self.ident = P.sb([128, 128], BF16, "ident")
            self.identbig = P.sb([128, 128], BF16, "identbig")
            self.cos2 = P.sb([128, TT, 16], F32, "cos2")
            self.sin2 = P.sb([128, TT, 16], F32, "sin2")
            self.tz = P.sb([128, 512], BF16, "tz")
            self.za = P.sb([128, 512], BF16, "za")
            self.gvec = P.sb([128, D], F32, "gvec")
            self.zeros = P.sb([128, 512], BF16, "zeros")
            P.op("pool", lambda e: e.memset(self.zeros[:], 0.0), [], [self.zeros])
            self.hT = [P.sb([128, 8, 512], BF16, "hT") for _ in range(4)]
            self.onT = P.sb([128, 4, S], BF16, "onT")
            self.odT = P.sb([128, 4, S], BF16, "odT")
            self.ar = Arena(P, 128 * 1024)
            P.dma("sp", self.ident[:], self.cin["c_ident"], writes=[self.ident])
            P.dma("sp", self.identbig[:], self.cin["c_identbig"], writes=[self.identbig])
            self.dma_tok(self.cos2, self.cin["c_cos2"])
            self.dma_tok(self.sin2, self.cin["c_sin2"])
            P.op("pool", lambda e: e.memset(self.tz[:], 0.0), [], [self.tz])
            P.op("pool", lambda e: e.memset(self.za[:], 0.0), [], [self.za])
            P.dma("sp", self.tz[:, 0:128], self.cin["c_tri"], writes=[self.tz])
            P.dma("sp", self.za[:, 384:512], self.cin["c_anti"], writes=[self.za])

            for b in range(nb):
                for l in range(nl):
                    self.layer(b, l)
                if self.want("F"):
                    self.phase_final(b)
                    P.barrier()
            P.barrier()
            P.build()
        return nc

    def want(self, ph):
        return self.phases is None or ph in self.phases

    def layer(self, b, l):
        P = self.P
        src = self.x_in[b] if l == 0 else self.scr["xs"]
        srcT = None if l == 0 else self.xsT
        if self.want("A") or self.want("a"):
            self.phase_norm(src, srcT, self.w["attn_norm"][l])
            if self.want("A"):
                self.phase_proj(l)
            P.barrier()
        if self.want("N"):
            self.phase_nsa(l)
            P.barrier()
        else:
            P.op("pool", lambda e: e.memset(self.onT[:], 0.0), [], [self.onT])
        if self.want("D"):
            self.phase_dsa(l)
            P.barrier()
        else:
            P.op("pool", lambda e: e.memset(self.odT[:], 0.0), [], [self.odT])
        if self.want("M"):
            self.phase_merge(b, l, src, srcT)
            P.barrier()
        if self.want("F"):
            self.phase_norm(self.scr["xs"], self.xsT, self.w["ffn_norm"][l])
            self.phase_ffn(b, l)
            P.barrier()

    def phase_norm(self, src, srcT, gdram):
        P = self.P
        ar = self.ar
        ar.off = 0
        P.dma("sp", self.gvec[:], gdram.partition_broadcast(128), writes=[self.gvec])
        xt = [ar.alloc(128, [D], F32, "xt") for _ in range(3)]
        hn = [ar.alloc(128, [D], BF16, "hn") for _ in range(2)]
        junk = ar.alloc(128, [D], BF16, "junk")
        st = [ar.alloc(128, [4], F32, "st") for _ in range(2)]
        for i in range(TT):
            x = xt[i % 3]
            rd = [srcT[i]] if srcT is not None else []
            P.dma("sp", x[:], src[i * 128:(i + 1) * 128, :], reads=rd, writes=[x])
            s = st[i % 2]
            h = hn[i % 2]
            P.op("act", lambda e, x=x, s=s: e.activation(out=junk[:], in_=x[:], func=AF.Square, accum_out=s[:, 0:1]), [x], [junk, s])
            P.op("dve", lambda e, s=s: e.tensor_scalar(out=s[:, 1:2], in0=s[:, 0:1], scalar1=1.0 / D, scalar2=EPS, op0=ALU.mult, op1=ALU.add), [s], [s])
            P.op("act", lambda e, s=s: e.activation(out=s[:, 2:3], in_=s[:, 1:2], func=AF.Sqrt), [s], [s])
            P.op("dve", lambda e, s=s: e.reciprocal(out=s[:, 3:4], in_=s[:, 2:3]), [s], [s])
            P.op("dve", lambda e, x=x, s=s, h=h: e.scalar_tensor_tensor(out=h[:], in0=x[:], scalar=s[:, 3:4], in1=self.gvec[:], op0=ALU.mult, op1=ALU.mult), [x, s, self.gvec], [h])
            pb = self.PS[4 + (i % 2)]
            pbv = pb.h.bitcast(BF16)
            for kc in range(8):
                P.op("pe", lambda e, h=h, kc=kc, pbv=pbv: e.transpose(pbv[:, kc * 128:(kc + 1) * 128], h[:, kc * 128:(kc + 1) * 128], self.ident[:]), [h, self.ident], [pb])
            ht = self.hT[i // 4]
            eng = "act" if i % 2 == 0 else "dve"
            dst = ht[:, :, (i % 4) * 128:(i % 4 + 1) * 128]
            srcv = pbv[:, 0:1024].rearrange("p (k t) -> p k t", k=8)
            if eng == "act":
                P.op("act", lambda e, dst=dst, srcv=srcv: e.copy(out=dst, in_=srcv), [pb], [ht])
            else:
                P.op("dve", lambda e, dst=dst, srcv=srcv: e.tensor_copy(out=dst, in_=srcv), [pb], [ht])

    def load_w(self, dst, src_ap, kparts=128):
        self.P.dma("pool", dst[:], src_ap.rearrange("(kc p) c -> p kc c", p=kparts), writes=[dst])

    def phase_proj(self, l):
        P = self.P
        ar = self.ar
        wbuf = [ar.alloc(128, [8, 512], BF16, "wb") for _ in range(3)]
        tokb = [ar.alloc(128, [512], BF16, "tokb") for _ in range(3)]
        t1 = [ar.alloc(128, [8, 16], F32, "t1") for _ in range(2)]
        t2 = [ar.alloc(128, [8, 16], F32, "t2") for _ in range(2)]
        stg = [ar.alloc(128, [4, 512], BF16, "stg") for _ in range(2)]
        sm = [ar.alloc(128, [32], F32, "sm") for _ in range(2)]
        chunks = proj_chunks()
        win = self.w["w_in"][l]
        cnt = 0
        import os
        sel = os.environ.get('KCH')
        def load_w(ci):
            c0, nc_, blocks = chunks[ci]
            wb = wbuf[ci % 3]
            P.dma("pool", wb[:, :, 0:nc_], win[:, c0:c0 + nc_].rearrange("(kc p) c -> p kc c", p=128), writes=[wb])

        def item(ci, i, cnt):
            c0, nc_, blocks = chunks[ci]
            wb = wbuf[ci % 3]
            if i == 0:
                if ci == 0:
                    load_w(0)
                if ci + 1 < len(chunks):
                    load_w(ci + 1)
            if True:
                ps = self.PS[cnt % 2]
                tb = tokb[cnt % 3]
                a1 = t1[cnt % 2]
                a2 = t2[cnt % 2]
                sg = stg[(i // 4) % 2]
                smt = sm[cnt % 2]
                cnt += 1
                ht = self.hT[i // 4]
                for kc in range(8):
                    P.op("pe", lambda e, ps=ps, ht=ht, kc=kc, i=i, wb=wb, nc_=nc_: e.matmul(ps[:, 0:nc_], lhsT=ht[:, kc, (i % 4) * 128:(i % 4 + 1) * 128], rhs=wb[:, kc, 0:nc_], start=(kc == 0), stop=(kc == 7)), [ht, wb], [ps])
                P.op("act", lambda e, tb=tb, ps=ps, nc_=nc_: e.copy(out=tb[:, 0:nc_], in_=ps[:, 0:nc_]), [ps], [tb])
                rblocks = [blk for blk in blocks if blk[3]]
                if rblocks and not os.environ.get('KNOROPE'):
                    nhr = (rblocks[-1][0] + rblocks[-1][1]) // 64
                    pv = ps[:, 0:nhr * 64].rearrange("p (h d) -> p h d", d=64)
                    cosb = self.cos2[:, i, :].unsqueeze(1).to_broadcast([128, nhr, 16])
                    sinlo = self.sin2[:, i, 0:8].unsqueeze(1).to_broadcast([128, nhr, 8])
                    sinhi = self.sin2[:, i, 8:16].unsqueeze(1).to_broadcast([128, nhr, 8])
                    P.op("dve", lambda e, a1=a1, pv=pv, cosb=cosb, nhr=nhr: e.tensor_tensor(out=a1[:, 0:nhr, :], in0=pv[:, :, 0:16], in1=cosb, op=ALU.mult), [ps, self.cos2], [a1])
                    P.op("dve", lambda e, a2=a2, pv=pv, sinlo=sinlo, nhr=nhr: e.tensor_tensor(out=a2[:, 0:nhr, 0:8], in0=pv[:, :, 8:16], in1=sinlo, op=ALU.mult), [ps, self.sin2], [a2])
                    P.op("dve", lambda e, a2=a2, pv=pv, sinhi=sinhi, nhr=nhr: e.tensor_tensor(out=a2[:, 0:nhr, 8:16], in0=pv[:, :, 0:8], in1=sinhi, op=ALU.mult), [ps, self.sin2], [a2])
                    runs = []
                    for (off, wdt, kind, rope, dest) in rblocks:
                        h0, h1 = off // 64, (off + wdt) // 64
                        if runs and runs[-1][1] == h0:
                            runs[-1][1] = h1
                        else:
                            runs.append([h0, h1])
                    for (h0, h1) in runs:
                        tv = tb[:, h0 * 64:h1 * 64].rearrange("p (h d) -> p h d", d=64)
                        P.op("dve", lambda e, a1=a1, a2=a2, tv=tv, h0=h0, h1=h1: e.tensor_tensor(out=tv[:, :, 0:16], in0=a1[:, h0:h1, :], in1=a2[:, h0:h1, :], op=ALU.add), [a1, a2, tb], [tb])
                yield
                pt = self.PS[2 + (cnt % 2)]
                ptv = pt.h.bitcast(BF16)
                nT = 0
                for bi, (off, wdt, kind, rope, dest) in enumerate(blocks):
                    dn, dh = dest
                    if kind == "T" and os.environ.get('KNOT'):
                        pass
                    elif kind == "T":
                        P.op("pe", lambda e, ptv=ptv, bi=bi, tb=tb, off=off, wdt=wdt: e.transpose(ptv[0:wdt, bi * 128:(bi + 1) * 128], tb[:, off:off + wdt], self.ident[:]), [tb, self.ident], [pt])
                        nT += 1
                    elif kind == "tok" and not os.environ.get('KNOOUT'):
                        P.dma("sp", self.scr[dn][i * 128:(i + 1) * 128, dh * 64:dh * 64 + wdt], tb[:, off:off + wdt], reads=[tb], writes=[self.scrT[dn]])
                    elif kind == "sig":
                        P.op("act", lambda e, smt=smt, ps=ps, off=off, wdt=wdt: e.activation(out=smt[:, 0:wdt], in_=ps[:, off:off + wdt], func=AF.Sigmoid), [ps], [smt])
                        P.dma("sp", self.scr[dn][i * 128:(i + 1) * 128, :], smt[:, 0:wdt], reads=[smt], writes=[self.scrT[dn]])
                    elif kind == "scale":
                        P.op("dve", lambda e, smt=smt, ps=ps, off=off, wdt=wdt: e.tensor_scalar(out=smt[:, 0:wdt], in0=ps[:, off:off + wdt], scalar1=IDXS, scalar2=None, op0=ALU.mult), [ps], [smt])
                        P.dma("sp", self.scr[dn][i * 128:(i + 1) * 128, :], smt[:, 0:wdt], reads=[smt], writes=[self.scrT[dn]])
                tblocks = [(bi, blk) for bi, blk in enumerate(blocks) if blk[2] == "T"]
                if tblocks and not os.environ.get('KNOT'):
                    full = all(blk[1] == 128 for _, blk in tblocks)
                    b0 = tblocks[0][0]
                    b1 = tblocks[-1][0] + 1
                    contiguous = [bi for bi, _ in tblocks] == list(range(b0, b1))
                    if full and contiguous:
                        srcv = ptv[:, b0 * 128:b1 * 128].rearrange("p (b t) -> p b t", t=128)
                        dstv = sg[:, b0:b1, (i % 4) * 128:(i % 4 + 1) * 128]
                        P.op("dve", lambda e, dstv=dstv, srcv=srcv: e.tensor_copy(out=dstv, in_=srcv), [pt], [sg])
                    else:
                        for bi, blk in tblocks:
                            wdt = blk[1]
                            P.op("dve", lambda e, sg=sg, ptv=ptv, bi=bi, wdt=wdt, i=i: e.tensor_copy(out=sg[0:wdt, bi, (i % 4) * 128:(i % 4 + 1) * 128], in_=ptv[0:wdt, bi * 128:(bi + 1) * 128]), [pt], [sg])
                    if i % 4 == 3:
                        tc = i // 4
                        for bi, blk in tblocks:
                            off, wdt, kind, rope, (dn, dh) = blk
                            nh = max(wdt // 64, 1)
                            dst = self.scr[dn][dh:dh + nh, :, tc * 512:(tc + 1) * 512].rearrange("h d t -> (h d) t")
                            P.dma("sp", dst, sg[0:wdt, bi, :], reads=[sg], writes=[self.scrT[dn]])


        items = []
        k = 0
        for ci in range(len(chunks)):
            for i in range(TT):
                items.append(item(ci, i, k))
                k += 1
        for k in range(len(items)):
            next(items[k])
            if k >= 1:
                for _ in items[k - 1]:
                    pass
        for _ in items[-1]:
            pass

    def phase_nsa(self, l):
        P = self.P
        ar = self.ar
        ar.off = 0
        qn = ar.alloc(96, [8, S], BF16, "qn")
        onsa = ar.alloc(128, [TT, 512], F32, "onsa")
        kcc = ar.alloc(64, [2, 128], BF16, "kcc")
        vca = ar.alloc(127, [2, 97], BF16, "vca")
        g24 = ar.alloc(128, [TT, 24], F32, "g24")
        self.cmask = ar.alloc(127, [S], BF16, "cmask")
        self.mimp = ar.alloc(128, [TT, 32], F32, "mimp")
        self.fbias = ar.alloc(128, [TT, 32], F32, "fbias")
        P.dma("sp", self.cmask[:], self.cin["c_cmask"], writes=[self.cmask])
        self.dma_tok(self.mimp, self.cin["c_mimp"])
        self.dma_tok(self.fbias, self.cin["c_fbias"])
        mark = ar.off
        for h in range(8):
            P.dma("sp", qn[0:64, h, :], self.scr["qnT"][h], reads=[self.scrT["qnT"]], writes=[qn])
        self.dma_tok(g24, self.scr["g24"], reads=[self.scrT["g24"]])
        P.op("pool", lambda e: e.memset(vca[:], 1.0), [], [vca])
        for g in range(2):
            P.dma("sp", vca[:, g, 65:97], self.cin["c_ov"], writes=[vca])
        kvT = ar.alloc(64, [2, S], BF16, "kvT")
        w1 = ar.alloc(64, [32, 128], BF16, "w1")
        w2 = ar.alloc(128, [64], BF16, "w2")
        posT = ar.alloc(64, [32], BF16, "posT")
        posn = ar.alloc(32, [64], BF16, "posn")
        b1 = ar.alloc(128, [1], F32, "b1")
        b2c = ar.alloc(64, [1], F32, "b2c")
        b2r = ar.alloc(127, [64], F32, "b2r")
        cb = ar.alloc(128, [2], F32, "cb")
        xs_ = ar.alloc(128, [128], F32, "gx")
        x2 = ar.alloc(128, [128], F32, "gx2")
        th = ar.alloc(128, [128], F32, "gth")
        gl = ar.alloc(128, [128], BF16, "gl")
        for kind in ("k", "v"):
            pre = "cmp%s_" % kind
            P.dma("sp", kvT[:], self.scr["kcT" if kind == "k" else "vcT"].rearrange("g d t -> d g t"), reads=[self.scrT["kcT" if kind == "k" else "vcT"]], writes=[kvT])
            P.dma("pool", w1[:], self.w[pre + "w1"][l].rearrange("(j d) h -> d j h", d=64), writes=[w1])
            P.dma("pool", w2[:], self.w[pre + "w2"][l], writes=[w2])
            P.dma("pool", posn[:], self.w[pre + "pos"][l], writes=[posn])
            ppos = self.PS[7]
            pposv = ppos.h.bitcast(BF16)
            P.op("pe", lambda e, pposv=pposv: e.transpose(pposv[0:64, 0:32], posn[:], self.ident[0:32, 0:32]), [posn, self.ident], [ppos])
            P.op("act", lambda e, pposv=pposv: e.copy(out=posT[:], in_=pposv[0:64, 0:32]), [ppos], [posT])
            P.dma("sp", b1[:], self.w[pre + "b1"][l].rearrange("(h o) -> h o", o=1), writes=[b1])
            P.dma("sp", b2c[:], self.w[pre + "b2"][l].rearrange("(h o) -> h o", o=1), writes=[b2c])
            P.dma("sp", b2r[:], self.w[pre + "b2"][l].partition_broadcast(127), writes=[b2r])
            pc = self.PS[4]
            for j in range(32):
                P.op("pe", lambda e, j=j: e.matmul(pc[:, 0:1], lhsT=w1[:, j, :], rhs=posT[:, j:j + 1], start=(j == 0), stop=(j == 31)), [w1, posT], [pc])
            P.op("dve", lambda e: e.tensor_tensor(out=cb[:, 0:1], in0=pc[:, 0:1], in1=b1[:], op=ALU.add), [pc, b1], [cb])
            for g in range(2):
                ph = self.PS[5]
                for j in range(32):
                    P.op("pe", lambda e, j=j, g=g: e.matmul(ph[:, 0:127], lhsT=w1[:, j, :], rhs=kvT[:, g, j:j + 16 * 126 + 1:16], start=(j == 0), stop=(j == 31)), [w1, kvT], [ph])
                P.op("act", lambda e: e.activation(out=xs_[:, 0:127], in_=ph[:, 0:127], func=AF.Identity, bias=cb[:, 0:1]), [ph, cb], [xs_])
                P.op("dve", lambda e: e.tensor_tensor(out=x2[:, 0:127], in0=xs_[:, 0:127], in1=xs_[:, 0:127], op=ALU.mult), [xs_], [x2])
                P.op("dve", lambda e: e.tensor_scalar(out=x2[:, 0:127], in0=x2[:, 0:127], scalar1=0.044715, scalar2=1.0, op0=ALU.mult, op1=ALU.add), [x2], [x2])
                P.op("dve", lambda e: e.tensor_tensor(out=x2[:, 0:127], in0=x2[:, 0:127], in1=xs_[:, 0:127], op=ALU.mult), [x2, xs_], [x2])
                P.op("act", lambda e: e.activation(out=th[:, 0:127], in_=x2[:, 0:127], func=AF.Tanh, scale=0.7978845608028654), [x2], [th])
                P.op("dve", lambda e: e.tensor_scalar(out=th[:, 0:127], in0=th[:, 0:127], scalar1=1.0, scalar2=0.5, op0=ALU.add, op1=ALU.mult), [th], [th])
                P.op("dve", lambda e: e.tensor_tensor(out=gl[:, 0:127], in0=th[:, 0:127], in1=xs_[:, 0:127], op=ALU.mult), [th, xs_], [gl])
                po = self.PS[6]
                if kind == "k":
                    P.op("pe", lambda e: e.matmul(po[0:64, 0:127], lhsT=w2[:], rhs=gl[:, 0:127], start=True, stop=True), [w2, gl], [po])
                    P.op("act", lambda e, g=g: e.activation(out=kcc[:, g, 0:127], in_=po[0:64, 0:127], func=AF.Identity, bias=b2c[:, 0:1]), [po, b2c], [kcc])
                else:
                    P.op("pe", lambda e: e.matmul(po[0:127, 0:64], lhsT=gl[:, 0:127], rhs=w2[:], start=True, stop=True), [w2, gl], [po])
                    P.op("dve", lambda e, g=g: e.tensor_tensor(out=vca[:, g, 0:64], in0=po[0:127, 0:64], in1=b2r[:], op=ALU.add), [po, b2r], [vca])
        if os.environ.get('KNSTOP') == '1':
            return
        P.barrier()
        ar.off = mark
        ks = ar.alloc(96, [2, S], BF16, "ks")
        kw = ar.alloc(64, [2, S], BF16, "kw")
        vs = ar.alloc(128, [TT, 2, 65], BF16, "vs")
        vw = ar.alloc(128, [TT, 2, 65], BF16, "vw")
        PT = [ar.alloc(128, [512], BF16, "PT") for _ in range(4)]
        ocn = ar.alloc(128, [4, 97], F32, "ocn")
        rdn = [ar.alloc(128, [4], F32, "rdn") for _ in range(4)]
        imp = ar.alloc(128, [32], F32, "imp")
        scr1 = ar.alloc(128, [32], F32, "scr1")
        scr2 = ar.alloc(128, [32], F32, "scr2")
        m8 = ar.alloc(128, [16], F32, "m8")
        nbw = ar.alloc(128, [96], BF16, "nbw")
        tmp = [ar.alloc(128, [4, 64], F32, "tmp") for _ in range(4)]
        P.dma("sp", ks[0:64, :, :], self.scr["ksT"].rearrange("g d t -> d g t"), reads=[self.scrT["ksT"]], writes=[ks])
        for g in range(2):
            P.dma("sp", ks[64:96, g, :], self.cin["c_E"], writes=[ks])
        P.dma("sp", kw[:], self.scr["kwT"].rearrange("g d t -> d g t"), reads=[self.scrT["kwT"]], writes=[kw])
        P.op("pool", lambda e: e.memset(vs[:], 1.0), [], [vs])
        P.op("pool", lambda e: e.memset(vw[:], 1.0), [], [vw])
        P.op("pool", lambda e: e.memset(nbw[:], 0.0), [], [nbw])
        for i in range(TT):
            P.dma("sp", vs[:, i, :, 0:64], self.scr["vs"][i * 128:(i + 1) * 128, :].rearrange("p (h d) -> p h d", d=64), reads=[self.scrT["vs"]], writes=[vs])
            P.dma("sp", vw[:, i, :, 0:64], self.scr["vw"][i * 128:(i + 1) * 128, :].rearrange("p (h d) -> p h d", d=64), reads=[self.scrT["vw"]], writes=[vw])
        kk = 0
        for g in range(2):
            for c in range(4):
                for r in range(4):
                    h = g * 4 + r
                    ps = self.PS[r % 2]
                    pt = PT[r]
                    P.op("pe", lambda e, ps=ps, c=c: e.matmul(ps[0:127, :], lhsT=self.identbig[0:127, 0:127], rhs=self.cmask[:, c * 512:(c + 1) * 512], start=True, stop=False), [self.identbig, self.cmask], [ps])
                    P.op("pe", lambda e, ps=ps, c=c, g=g, h=h: e.matmul(ps[0:127, :], lhsT=kcc[:, g, 0:127], rhs=qn[0:64, h, c * 512:(c + 1) * 512], start=False, stop=True), [kcc, qn], [ps])
                    P.op("act", lambda e, ps=ps, pt=pt: e.activation(out=pt[0:127, :], in_=ps[0:127, :], func=AF.Exp, scale=SCALE), [ps], [pt])
                for il in range(4):
                    i = 4 * c + il
                    oc = self.PS[2 + (kk % 2)]
                    rd = rdn[kk % 2]
                    kk += 1
                    for r in range(4):
                        P.op("pe", lambda e, oc=oc, r=r, il=il, g=g: e.matmul(oc[:, r * 97:(r + 1) * 97], lhsT=PT[r][0:127, il * 128:(il + 1) * 128], rhs=vca[:, g, :], start=True, stop=True), [PT[r], vca], [oc])
                    ov = oc[:, 0:388].rearrange("p (r c) -> p r c", c=97)
                    P.op("dve", lambda e, rd=rd, ov=ov: e.tensor_scalar(out=rd[:], in0=ov[:, :, 64], scalar1=1e-30, scalar2=None, op0=ALU.max), [oc], [rd])
                    P.op("dve", lambda e, rd=rd: e.reciprocal(out=rd[:], in_=rd[:]), [rd], [rd])
                    P.op("dve", lambda e, rd=rd, ov=ov: e.tensor_tensor(out=ocn[:], in0=ov, in1=rd[:].unsqueeze(2).to_broadcast([128, 4, 97]), op=ALU.mult), [oc, rd], [ocn])
                    P.op("dve", lambda e, i=i, g=g: e.tensor_tensor(out=onsa[:, i, g * 256:(g + 1) * 256].rearrange("p (r d) -> p r d", d=64), in0=ocn[:, :, 0:64], in1=g24[:, i, g * 4:g * 4 + 4].unsqueeze(2).to_broadcast([128, 4, 64]), op=ALU.mult), [ocn, g24], [onsa])
                    P.op("dve", lambda e: e.tensor_reduce(out=imp[:], in_=ocn[:, :, 65:97].rearrange("p r j -> p j r"), axis=AX.X, op=ALU.add), [ocn], [imp])
                    P.op("dve", lambda e, i=i: e.tensor_tensor(out=scr1[:], in0=imp[:], in1=self.mimp[:, i, :], op=ALU.mult), [imp, self.mimp], [scr1])
                    P.op("dve", lambda e, i=i: e.tensor_tensor(out=scr1[:], in0=scr1[:], in1=self.fbias[:, i, :], op=ALU.add), [scr1, self.fbias], [scr1])
                    P.op("dve", lambda e: e.max(out=m8[:, 0:8], in_=scr1[:]), [scr1], [m8])
                    P.op("dve", lambda e: e.match_replace(out=scr2[:], in_to_replace=m8[:, 0:8], in_values=scr1[:], imm_value=NEG), [m8, scr1], [scr2])
                    P.op("dve", lambda e: e.max(out=m8[:, 8:16], in_=scr2[:]), [scr2], [m8])
                    P.op("dve", lambda e: e.tensor_scalar(out=nbw[:, 64:96], in0=scr1[:], scalar1=m8[:, 15:16], scalar2=-1.0, op0=ALU.is_ge, op1=ALU.add), [scr1, m8], [nbw])
                    pt2 = self.PS[4 + (kk % 2)]
                    ptv = pt2.h.bitcast(BF16)
                    P.op("pe", lambda e, ptv=ptv: e.transpose(ptv[0:96, 0:128], nbw[:], self.ident[:]), [nbw, self.ident], [pt2])
                    P.op("act", lambda e, ptv=ptv, g=g, i=i: e.copy(out=qn[64:96, g * 4:(g + 1) * 4, i * 128:(i + 1) * 128], in_=ptv[64:96, 0:128].unsqueeze(1).to_broadcast([32, 4, 128])), [pt2], [qn])
        if os.environ.get('KNSTOP') == '2':
            return
        def nsa_head(br, h, slot):
            kt_, vt_, kd = [(ks, vs, 96), (kw, vw, 64)][br]
            g = h // 4
            cnt_ = [0]

            def out_cb(c, osb, h=h, br=br):
                ov = osb[:, 0:260].rearrange("p (a b) -> p a b", b=65)
                rd = rdn[slot * 2 + cnt_[0] % 2]
                tp = tmp[slot * 2 + cnt_[0] % 2]
                cnt_[0] += 1
                P.op("dve", lambda e, rd=rd, ov=ov: e.reciprocal(out=rd[:], in_=ov[:, :, 64]), [osb], [rd])
                P.op("dve", lambda e, rd=rd, c=c, h=h, br=br: e.tensor_tensor(out=rd[:], in0=rd[:], in1=g24[:, 4 * c:4 * c + 4, (br + 1) * 8 + h], op=ALU.mult), [rd, g24], [rd])
                P.op("dve", lambda e, rd=rd, ov=ov, tp=tp: e.tensor_tensor(out=tp[:], in0=ov[:, :, 0:64], in1=rd[:].unsqueeze(2).to_broadcast([128, 4, 64]), op=ALU.mult), [osb, rd], [tp])
                P.op("pool", lambda e, tp=tp, c=c, h=h: e.tensor_tensor(out=onsa[:, 4 * c:4 * c + 4, h * 64:(h + 1) * 64], in0=onsa[:, 4 * c:4 * c + 4, h * 64:(h + 1) * 64], in1=tp[:], op=ALU.add), [tp, onsa], [onsa])

            if br == 0:
                pairs = lambda i: list(range(0, i + 1))
            else:
                pairs = lambda i: list(range(max(0, i - 4), i + 1))

            def nbias(c, j, valid):
                n = len(valid) * 128
                if valid[0] == j:
                    return (self.tz[:, 0:n], self.tz)
                if valid[-1] == j + 4:
                    return (self.za[:, 512 - n:512], self.za)
                return None

            return self.attn_head(lambda j, kt_=kt_, g=g, kd=kd: kt_[0:kd, g, j * 128:(j + 1) * 128],
                                  lambda i0_, i1_, h=h, kd=kd: qn[0:kd, h, i0_ * 128:i1_ * 128],
                                  lambda j, vt_=vt_, g=g: vt_[:, j, g, :], [qn, kt_, vt_],
                                  pairs, nbias, out_cb, PTn[3 * slot:3 * slot + 3], slot=slot)

        PTn = PT + [ar.alloc(128, [512], BF16, "PTx") for _ in range(2)]
        for br in range(2):
            if br == 1 and os.environ.get('KNSTOP') == '3':
                return
            for hp in range(4):
                self.interleave([nsa_head(br, 2 * hp, 0), nsa_head(br, 2 * hp + 1, 1)])
        onb = ar.alloc(128, [TT, 512], BF16, "onb")
        P.op("act", lambda e: e.copy(out=onb[:], in_=onsa[:]), [onsa], [onb])
        self.to_featmajor(onb, self.onT)
        if self.dbg:
            self.dscr_once("dbg_onsa", [S, 512], BF16)
            P.dma("sp", self.scr["dbg_onsa"].rearrange("(i p) c -> p i c", p=128), onb[:], reads=[onb], writes=[self.scrT["dbg_onsa"]])

    def attn_head(self, KT, QTr, V, rds, pairs, bias_fn, out_cb, PT, slot=0, mask_fn=None):
        P = self.P
        sbanks = [self.PS[0], self.PS[1]] if slot == 0 else [self.PS[6], self.PS[7]]
        obanks = [self.PS[2], self.PS[3]] if slot == 0 else [self.PS[4], self.PS[5]]
        scnt = 0
        ocnt = 0
        for c in range(4):
            tiles = list(range(4 * c, 4 * c + 4))
            js = sorted(set(j for i in tiles for j in pairs(i)))
            osb = obanks[ocnt % 2]
            ocnt += 1
            state = {}

            def emit_S(j, c=c, tiles=tiles):
                nonlocal scnt
                valid = [i for i in tiles if j in pairs(i)]
                ps = sbanks[scnt % 2]
                pt = PT[scnt % len(PT)]
                scnt += 1
                lo = (valid[0] - 4 * c) * 128
                hi = (valid[-1] - 4 * c + 1) * 128
                bz = bias_fn(c, j, valid)
                if bz is not None:
                    bap, bt = bz
                    P.op("pe", lambda e, ps=ps, lo=lo, hi=hi, bap=bap: e.matmul(ps[:, lo:hi], lhsT=self.identbig[:], rhs=bap, start=True, stop=False), [self.identbig, bt], [ps])
                P.op("pe", lambda e, ps=ps, lo=lo, hi=hi, kt_=KT(j), qt_=QTr(valid[0], valid[-1] + 1), nb_=(bz is None): e.matmul(ps[:, lo:hi], lhsT=kt_, rhs=qt_, start=nb_, stop=True), rds, [ps])
                P.op("act", lambda e, ps=ps, pt=pt, lo=lo, hi=hi: e.activation(out=pt[:, lo:hi], in_=ps[:, lo:hi], func=AF.Exp, scale=SCALE), [ps], [pt])
                if mask_fn is not None:
                    map_, mt = mask_fn(c, j, valid)
                    P.op("dve", lambda e, pt=pt, lo=lo, hi=hi, map_=map_: e.tensor_tensor(out=pt[:, lo:hi], in0=pt[:, lo:hi], in1=map_, op=ALU.mult), [pt, mt], [pt])
                state[j] = (valid, pt)

            def emit_PV(j, c=c, js=js, osb=osb):
                valid, pt = state[j]
                for i in valid:
                    c0 = (i - 4 * c) * 128
                    o0 = (i - 4 * c) * 65
                    sp_ = (j == js[-1] and i == valid[-1])
                    P.op("pe", lambda e, pt=pt, c0=c0, o0=o0, osb=osb, v_=V(j), sp_=sp_: e.matmul(osb[:, o0:o0 + 65], lhsT=pt[:, c0:c0 + 128], rhs=v_, start=False, stop=sp_), [pt] + rds, [osb])

            P.op("pe", lambda e, osb=osb: e.matmul(osb[:, 0:260], lhsT=self.zeros[:, 0:128], rhs=self.zeros[:, 0:260], start=True, stop=False), [self.zeros], [osb])
            emit_S(js[0])
            yield
            for idx, j in enumerate(js):
                if idx + 1 < len(js):
                    emit_S(js[idx + 1])
                emit_PV(j)
                yield
            out_cb(c, osb)
            yield

    @staticmethod
    def interleave(gens):
        gens = list(gens)
        while gens:
            for g in list(gens):
                try:
                    next(g)
                except StopIteration:
                    gens.remove(g)

    def phase_dsa(self, l):
        P = self.P
        ar = self.ar
        ar.off = 0
        if "mbT" not in self.scr:
            self.dscr("mbT", [136, 128, 128], BF16)
            self.c_triT = P.sb([128, 128], F32, "triT")
            P.dma("sp", self.c_triT[:], self.cin["c_triT"], writes=[self.c_triT])
        mbT_d = self.scr["mbT"]
        mbT_T = self.scrT["mbT"]
        mbT = ar.alloc(128, [136, 128], BF16, "mbT")
        mbT_end = ar.off
        ar.off = 0
        iqT = ar.alloc(64, [8, S], BF16, "iqT")
        ikT = ar.alloc(64, [S], BF16, "ikT")
        assert ar.off >= mbT_end
        iwt = ar.alloc(128, [TT, 8], F32, "iwt")
        dg = [ar.alloc(128, [8, 128], BF16, "dg") for _ in range(2)]
        rb = [ar.alloc(128, [512], BF16, "rb") for _ in range(4)]
        sc = [ar.alloc(128, [128 * (i + 1)], F32, "sc") for i in range(TT)]
        thrc = ar.alloc(128, [TT], F32, "thrc")
        nmid = ar.alloc(128, [TT], F32, "nmid")
        cnt2 = ar.alloc(128, [TT], F32, "cnt2")
        lo = ar.alloc(128, [TT], F32, "lo")
        wv = ar.alloc(128, [TT], F32, "wv")
        mid = ar.alloc(128, [TT], F32, "mid")
        cnt = ar.alloc(128, [TT], F32, "cnt")
        ge = ar.alloc(128, [TT], F32, "ge")
        m8 = ar.alloc(128, [TT, 8], F32, "m8")
        mb = [ar.alloc(128, [S], BF16, "mb") for _ in range(2)]
        P.dma("sp", iqT[:], self.scr["iqT"].rearrange("h d t -> d h t"), reads=[self.scrT["iqT"]], writes=[iqT])
        P.dma("sp", ikT[:], self.scr["ikT"][0], reads=[self.scrT["ikT"]], writes=[ikT])
        self.dma_tok(iwt, self.scr["iw"], reads=[self.scrT["iw"]])
        ka = 0
        kb = 0
        kr = 0
        for i in range(TT):
            d = dg[i % 2]
            identb = self.ident[:].unsqueeze(1).to_broadcast([128, 8, 128])
            iwb = iwt[:, i, :].unsqueeze(2).to_broadcast([128, 8, 128])
            P.op("dve", lambda e, d=d, identb=identb, iwb=iwb: e.tensor_tensor(out=d[:], in0=identb, in1=iwb, op=ALU.mult), [self.ident, iwt], [d])
            L = 128 * (i + 1)
            units = [(sci, h) for sci in range(i // 4 + 1) for h in range(8)]
            pAb = [self.PS[0], self.PS[1], self.PS[6], self.PS[7]]
            st_ = {}

            def emit_front(u, i=i, L=L):
                nonlocal ka, kr
                sci, h = units[u]
                ncol = min(512, L - 512 * sci)
                pA = pAb[ka % 4]
                ka += 1
                r = rb[kr % 4]
                kr += 1
                P.op("pe", lambda e, pA=pA, h=h, i=i, sci=sci, ncol=ncol: e.matmul(pA[:, 0:ncol], lhsT=iqT[:, h, i * 128:(i + 1) * 128], rhs=ikT[:, sci * 512:sci * 512 + ncol], start=True, stop=True), [iqT, ikT], [pA])
                if h % 2 == 0:
                    P.op("act", lambda e, pA=pA, r=r, ncol=ncol: e.activation(out=r[:, 0:ncol], in_=pA[:, 0:ncol], func=AF.Relu), [pA], [r])
                else:
                    P.op("dve", lambda e, pA=pA, r=r, ncol=ncol: e.tensor_scalar(out=r[:, 0:ncol], in0=pA[:, 0:ncol], scalar1=0.0, scalar2=None, op0=ALU.max), [pA], [r])
                st_[u] = (r, ncol)

            def emit_back(u, i=i, d=d):
                nonlocal kb
                sci, h = units[u]
                r, ncol = st_[u]
                pB = self.PS[2 + (kb % 2)]
                P.op("pe", lambda e, pB=pB, d=d, h=h, r=r, ncol=ncol: e.matmul(pB[:, 0:ncol], lhsT=d[:, h, :], rhs=r[:, 0:ncol], start=(h == 0), stop=(h == 7)), [d, r], [pB])
                if h == 7:
                    P.op("act", lambda e, pB=pB, i=i, sci=sci, ncol=ncol: e.copy(out=sc[i][:, sci * 512:sci * 512 + ncol], in_=pB[:, 0:ncol]), [pB], [sc[i]])
                    kb += 1

            emit_front(0)
            emit_front(1)
            for u in range(len(units)):
                if u + 2 < len(units):
                    emit_front(u + 2)
                emit_back(u)
            P.op("pool", lambda e, i=i: e.tensor_tensor(out=sc[i][:, i * 128:(i + 1) * 128], in0=sc[i][:, i * 128:(i + 1) * 128], in1=self.c_triT[:], op=ALU.add), [sc[i], self.c_triT], [sc[i]])
        if os.environ.get('KDSTOP') == '1':
            return
        P.op("dve", lambda e: e.memset(lo[:], -1e29), [], [lo])
        for i in range(2, TT):
            P.op("dve", lambda e, i=i: e.max(out=m8[:, i, :], in_=sc[i][:]), [sc[i]], [m8])
            P.op("dve", lambda e, i=i: e.tensor_reduce(out=lo[:, i:i + 1], in_=sc[i][:, 0:128 * i], axis=AX.X, op=ALU.min), [sc[i]], [lo])
        P.op("dve", lambda e: e.tensor_tensor(out=wv[:, 2:TT], in0=m8[:, 2:TT, 0], in1=lo[:, 2:TT], op=ALU.subtract), [m8, lo], [wv])
        ACT_TILES = (10, 11, 12, 13, 14, 15)
        P.op("dve", lambda e: e.memset(thrc[:], 255.5), [], [thrc])
        for i in ACT_TILES:
            P.op("dve", lambda e, i=i: e.memset(thrc[:, i:i + 1], 511.0 - 128.0 * (i + 1)), [], [thrc])
        for it in range(N_ITERS):
            P.op("dve", lambda e: e.tensor_scalar(out=wv[:, 2:TT], in0=wv[:, 2:TT], scalar1=0.5, scalar2=None, op0=ALU.mult), [wv], [wv])
            P.op("dve", lambda e: e.tensor_tensor(out=mid[:, 2:TT], in0=lo[:, 2:TT], in1=wv[:, 2:TT], op=ALU.add), [lo, wv], [mid])
            P.op("dve", lambda e: e.tensor_scalar(out=nmid[:, 2:TT], in0=mid[:, 2:TT], scalar1=-1.0, scalar2=None, op0=ALU.mult), [mid], [nmid])
            for i in range(TT - 1, 1, -1):
                if i in ACT_TILES:
                    P.op("act", lambda e, i=i: e.activation(out=mb[1][:, 0:128 * (i + 1)], in_=sc[i][:], func=AF.Sign, bias=nmid[:, i:i + 1], scale=1.0, accum_out=cnt[:, i:i + 1]), [sc[i], nmid], [mb[1], cnt])
            for i in range(2, TT):
                if i not in ACT_TILES:
                    P.op("dve", lambda e, i=i: e.tensor_scalar(out=mb[0][:, 0:128 * (i + 1)], in0=sc[i][:], scalar1=mid[:, i:i + 1], scalar2=None, op0=ALU.is_ge, op1=ALU.add, accum_out=cnt2[:, i:i + 1]), [sc[i], mid], [mb[0], cnt2])
            for i in range(2, TT):
                pass
            P.op("dve", lambda e: e.tensor_copy(out=cnt2[:, 10:16], in_=cnt[:, 10:16]), [cnt], [cnt2])
            P.op("dve", lambda e: e.tensor_tensor(out=ge[:, 2:TT], in0=cnt2[:, 2:TT], in1=thrc[:, 2:TT], op=ALU.is_ge), [cnt2, thrc], [ge])
            P.op("dve", lambda e: e.tensor_tensor(out=ge[:, 2:TT], in0=ge[:, 2:TT], in1=wv[:, 2:TT], op=ALU.mult), [ge, wv], [ge])
            P.op("dve", lambda e: e.tensor_tensor(out=lo[:, 2:TT], in0=lo[:, 2:TT], in1=ge[:, 2:TT], op=ALU.add), [lo, ge], [lo])
        if os.environ.get('KDSTOP') == '2':
            return
        P.barrier()
        kt = 0
        for i in range(TT):
            m = mb[i % 2]
            L = 128 * (i + 1)
            P.op("dve", lambda e, m=m, i=i, L=L: e.tensor_scalar(out=m[:, 0:L], in0=sc[i][:], scalar1=lo[:, i:i + 1], scalar2=None, op0=ALU.is_ge), [sc[i], lo], [m])
            for j0 in range(0, i + 1, 4):
                nbk = min(4, i + 1 - j0)
                pt = self.PS[4 + (kt % 2)]
                kt += 1
                ptv = pt.h.bitcast(BF16)
                for jj in range(nbk):
                    j = j0 + jj
                    P.op("pe", lambda e, ptv=ptv, jj=jj, m=m, j=j: e.transpose(ptv[:, jj * 128:(jj + 1) * 128], m[:, j * 128:(j + 1) * 128], self.ident[:]), [m, self.ident], [pt])
                for jj in range(nbk):
                    j = j0 + jj
                    bi_ = 16 * j - j * (j - 1) // 2 + (i - j)
                    if jj % 2 == 0:
                        P.op("act", lambda e, ptv=ptv, jj=jj, bi_=bi_: e.copy(out=mbT[:, bi_, :], in_=ptv[:, jj * 128:(jj + 1) * 128]), [pt], [mbT])
                    else:
                        P.op("dve", lambda e, ptv=ptv, jj=jj, bi_=bi_: e.tensor_copy(out=mbT[:, bi_, :], in_=ptv[:, jj * 128:(jj + 1) * 128]), [pt], [mbT])
        if self.dbg:
            self.dscr_once("dbg_thr", [128, TT], F32)
            P.dma("sp", self.scr["dbg_thr"], lo[:], reads=[lo], writes=[self.scrT["dbg_thr"]])
        if os.environ.get('KDSTOP') == '3':
            return
        P.barrier()
        ar.off = mbT_end
        vd = ar.alloc(128, [TT, 8, 65], BF16, "vdaug")
        qh = [ar.alloc(64, [S], BF16, "qh") for _ in range(4)]
        kh = [ar.alloc(64, [S], BF16, "kh") for _ in range(4)]
        PT = [ar.alloc(128, [512], BF16, "PT") for _ in range(6)]
        odsa = ar.alloc(128, [TT, 512], BF16, "odsa")
        rden = [ar.alloc(128, [4], F32, "rden") for _ in range(4)]
        P.op("pool", lambda e: e.memset(vd[:], 1.0), [], [vd])
        for i in range(TT):
            P.dma("sp", vd[:, i, :, 0:64], self.scr["vd"][i * 128:(i + 1) * 128, :].rearrange("p (h d) -> p h d", d=64), reads=[self.scrT["vd"]], writes=[vd])
        def dsa_head(h, slot):
            q = qh[h % 4]
            k_ = kh[h % 4]
            P.dma("sp", q[:], self.scr["qdT"][h], reads=[self.scrT["qdT"]], writes=[q])
            P.dma("sp", k_[:], self.scr["kdT"][h], reads=[self.scrT["kdT"]], writes=[k_])
            cnt_ = [0]

            def out_cb(c, osb, h=h):
                ov = osb[:, 0:260].rearrange("p (a b) -> p a b", b=65)
                rd = rden[slot * 2 + cnt_[0] % 2]
                cnt_[0] += 1
                P.op("dve", lambda e, rd=rd, ov=ov: e.reciprocal(out=rd[:], in_=ov[:, :, 64]), [osb], [rd])
                P.op("dve", lambda e, rd=rd, ov=ov, c=c, h=h: e.tensor_tensor(out=odsa[:, 4 * c:4 * c + 4, h * 64:(h + 1) * 64], in0=ov[:, :, 0:64], in1=rd[:].unsqueeze(2).to_broadcast([128, 4, 64]), op=ALU.mult), [osb, rd], [odsa])

            def dbias(c, j, valid):
                b0 = 16 * j - j * (j - 1) // 2 + (valid[0] - j)
                return (mbT[:, b0:b0 + len(valid), :].rearrange("p b t -> p (b t)"), mbT)

            return self.attn_head(lambda j, k_=k_: k_[:, j * 128:(j + 1) * 128], lambda i0_, i1_, q=q: q[:, i0_ * 128:i1_ * 128],
                                  lambda j, h=h: vd[:, j, h, :], [q, k_, vd],
                                  lambda i: list(range(0, i + 1)), lambda c, j, valid: None, out_cb, PT[3 * slot:3 * slot + 3], slot=slot, mask_fn=dbias)

        for hp in range(4):
            self.interleave([dsa_head(2 * hp, 0), dsa_head(2 * hp + 1, 1)])
        self.to_featmajor(odsa, self.odT)
        if self.dbg:
            self.dscr_once("dbg_odsa", [S, 512], BF16)
            P.dma("sp", self.scr["dbg_odsa"].rearrange("(i p) c -> p i c", p=128), odsa[:], reads=[odsa], writes=[self.scrT["dbg_odsa"]])

    def dma_tok(self, dst, src_dram, reads=()):
        v = src_dram.rearrange("(i p) c -> p i c", p=128)
        for q4 in range(4):
            self.P.dma("sp", dst[:, 4 * q4:4 * q4 + 4, :], v[:, 4 * q4:4 * q4 + 4, :], reads=list(reads), writes=[dst])

    def dscr_once(self, name, shape, dtype):
        if name not in self.scr:
            self.dscr(name, shape, dtype)

    def to_featmajor(self, tok, dstT):
        P = self.P
        for i in range(TT):
            pt = self.PS[4 + (i % 2)]
            ptv = pt.h.bitcast(BF16)
            for fc in range(4):
                P.op("pe", lambda e, ptv=ptv, fc=fc, i=i: e.transpose(ptv[:, fc * 128:(fc + 1) * 128], tok[:, i, fc * 128:(fc + 1) * 128], self.ident[:]), [tok, self.ident], [pt])
            P.op("act", lambda e, ptv=ptv, i=i: e.copy(out=dstT[:, :, i * 128:(i + 1) * 128], in_=ptv[:, 0:512].rearrange("p (f t) -> p f t", t=128)), [pt], [dstT])

    def phase_merge(self, b, l, src, srcT):
        P = self.P
        ar = self.ar
        ar.off = 0
        wbn = ar.alloc(128, [4, D], BF16, "wbn")
        wbd = ar.alloc(128, [4, D], BF16, "wbd")
        wo = ar.alloc(128, [8, D], BF16, "wo")
        wg = [ar.alloc(128, [8, 256], BF16, "wg") for _ in range(2)]
        mT = ar.alloc(128, [8, S], BF16, "mT")
        sg = [ar.alloc(128, [512], F32, "sg") for _ in range(8)]
        xt = [ar.alloc(128, [D], F32, "xt") for _ in range(2)]
        P.dma("pool", wbn[:], self.w["w_branch_nsa"][l].rearrange("(kc p) c -> p kc c", p=128), writes=[wbn])
        P.dma("pool", wbd[:], self.w["w_branch_dsa"][l].rearrange("(kc p) c -> p kc c", p=128), writes=[wbd])
        P.dma("pool", wo[:], self.w["w_out"][l].rearrange("(kc p) c -> p kc c", p=128), writes=[wo])
        win = self.w["w_in"][l]
        k = 0
        for cc in range(8):
            g = wg[cc % 2]
            P.dma("pool", g[:, :, 0:128], win[:, 3424 + cc * 128:3424 + (cc + 1) * 128].rearrange("(kc p) c -> p kc c", p=128), writes=[g])
            P.dma("pool", g[:, :, 128:256], win[:, 4448 + cc * 128:4448 + (cc + 1) * 128].rearrange("(kc p) c -> p kc c", p=128), writes=[g])
            for tc in range(4):
                bs_ = 4 * ((cc * 4 + tc) % 2)
                pyn, pyd, pgn, pgd = self.PS[bs_], self.PS[bs_ + 1], self.PS[bs_ + 2], self.PS[bs_ + 3]
                ts_ = slice(tc * 512, (tc + 1) * 512)
                for fc in range(4):
                    P.op("pe", lambda e, fc=fc, cc=cc, ts_=ts_, pyn=pyn: e.matmul(pyn[:], lhsT=wbn[:, fc, cc * 128:(cc + 1) * 128], rhs=self.onT[:, fc, ts_], start=(fc == 0), stop=(fc == 3)), [wbn, self.onT], [pyn])
                for fc in range(4):
                    P.op("pe", lambda e, fc=fc, cc=cc, ts_=ts_, pyd=pyd: e.matmul(pyd[:], lhsT=wbd[:, fc, cc * 128:(cc + 1) * 128], rhs=self.odT[:, fc, ts_], start=(fc == 0), stop=(fc == 3)), [wbd, self.odT], [pyd])
                ht = self.hT[tc]
                for kc in range(8):
                    P.op("pe", lambda e, kc=kc, g=g, ht=ht, pgn=pgn: e.matmul(pgn[:], lhsT=g[:, kc, 0:128], rhs=ht[:, kc, :], start=(kc == 0), stop=(kc == 7)), [g, ht], [pgn])
                for kc in range(8):
                    P.op("pe", lambda e, kc=kc, g=g, ht=ht, pgd=pgd: e.matmul(pgd[:], lhsT=g[:, kc, 128:256], rhs=ht[:, kc, :], start=(kc == 0), stop=(kc == 7)), [g, ht], [pgd])
                s1, s2, m1, m2 = sg[bs_:bs_ + 4]
                P.op("act", lambda e, s1=s1, pgn=pgn: e.activation(out=s1[:], in_=pgn[:], func=AF.Sigmoid), [pgn], [s1])
                P.op("act", lambda e, s2=s2, pgd=pgd: e.activation(out=s2[:], in_=pgd[:], func=AF.Sigmoid), [pgd], [s2])
                P.op("dve", lambda e, m1=m1, s1=s1, pyn=pyn: e.tensor_tensor(out=m1[:], in0=pyn[:], in1=s1[:], op=ALU.mult), [pyn, s1], [m1])
                P.op("dve", lambda e, m2=m2, s2=s2, pyd=pyd: e.tensor_tensor(out=m2[:], in0=pyd[:], in1=s2[:], op=ALU.mult), [pyd, s2], [m2])
                P.op("pool", lambda e, cc=cc, ts_=ts_, m1=m1, m2=m2: e.tensor_tensor(out=mT[:, cc, ts_], in0=m1[:], in1=m2[:], op=ALU.add), [m1, m2], [mT])
        for i in range(TT):
            x = xt[i % 2]
            rd = [srcT[i]] if srcT is not None else []
            P.dma("sp", x[:], src[i * 128:(i + 1) * 128, :], reads=rd, writes=[x])
            for half in range(2):
                ps = self.PS[4 + (k % 2)]
                k += 1
                for cc in range(8):
                    P.op("pe", lambda e, ps=ps, cc=cc, i=i, half=half: e.matmul(ps[:], lhsT=mT[:, cc, i * 128:(i + 1) * 128], rhs=wo[:, cc, half * 512:(half + 1) * 512], start=(cc == 0), stop=(cc == 7)), [mT, wo], [ps])
                P.op("dve", lambda e, ps=ps, x=x, half=half: e.tensor_tensor(out=x[:, half * 512:(half + 1) * 512], in0=ps[:], in1=x[:, half * 512:(half + 1) * 512], op=ALU.add), [ps, x], [x])
            P.dma("sp", self.scr["xs"][i * 128:(i + 1) * 128, :], x[:], reads=[x], writes=[self.xsT[i]])

    def phase_ffn(self, b, l):
        P = self.P
        ar = self.ar
        P.barrier()
        ar.off = 0
        actT = ar.alloc(128, [NFC, S], BF16, "actT")
        wgu = [ar.alloc(128, [8, 256], BF16, "wgu") for _ in range(2)]
        wd = ar.alloc(128, [NFC, 512], BF16, "wd")
        sgb = [ar.alloc(128, [512], F32, "sgb") for _ in range(2)]
        xt = [ar.alloc(128, [512], F32, "xh") for _ in range(2)]
        wgate = self.w["w_ffn_gate"][l]
        wup = self.w["w_ffn_up"][l]
        wdn = self.w["w_ffn_down"][l]
        k = 0
        for fc in range(NFC):
            g = wgu[fc % 2]
            P.dma("pool", g[:, :, 0:128], wgate[:, fc * 128:(fc + 1) * 128].rearrange("(kc p) c -> p kc c", p=128), writes=[g])
            P.dma("pool", g[:, :, 128:256], wup[:, fc * 128:(fc + 1) * 128].rearrange("(kc p) c -> p kc c", p=128), writes=[g])
            for tc in range(4):
                pg = self.PS[(k % 2) * 2]
                pu = self.PS[(k % 2) * 2 + 1]
                sb_ = sgb[k % 2]
                k += 1
                ht = self.hT[tc]
                for kc in range(8):
                    P.op("pe", lambda e, kc=kc, g=g, ht=ht, pg=pg: e.matmul(pg[:], lhsT=g[:, kc, 0:128], rhs=ht[:, kc, :], start=(kc == 0), stop=(kc == 7)), [g, ht], [pg])
                for kc in range(8):
                    P.op("pe", lambda e, kc=kc, g=g, ht=ht, pu=pu: e.matmul(pu[:], lhsT=g[:, kc, 128:256], rhs=ht[:, kc, :], start=(kc == 0), stop=(kc == 7)), [g, ht], [pu])
                P.op("act", lambda e, sb_=sb_, pg=pg: e.activation(out=sb_[:], in_=pg[:], func=AF.Silu), [pg], [sb_])
                P.op("dve", lambda e, sb_=sb_, pu=pu, fc=fc, tc=tc: e.tensor_tensor(out=actT[:, fc, tc * 512:(tc + 1) * 512], in0=pu[:], in1=sb_[:], op=ALU.mult), [pu, sb_], [actT])
        for half in range(2):
            P.dma("pool", wd[:], wdn[:, half * 512:(half + 1) * 512].rearrange("(kc p) c -> p kc c", p=128), writes=[wd])
            for i in range(TT):
                x = xt[i % 2]
                P.dma("sp", x[:], self.scr["xs"][i * 128:(i + 1) * 128, half * 512:(half + 1) * 512], reads=[self.xsT[i]], writes=[x])
                ps = self.PS[4 + (i % 2)]
                for fc in range(NFC):
                    P.op("pe", lambda e, ps=ps, fc=fc, i=i: e.matmul(ps[:], lhsT=actT[:, fc, i * 128:(i + 1) * 128], rhs=wd[:, fc, :], start=(fc == 0), stop=(fc == NFC - 1)), [actT, wd], [ps])
                P.op("dve", lambda e, ps=ps, x=x: e.tensor_tensor(out=x[:], in0=ps[:], in1=x[:], op=ALU.add), [ps, x], [x])
                P.dma("sp", self.scr["xs"][i * 128:(i + 1) * 128, half * 512:(half + 1) * 512], x[:], reads=[x], writes=[self.xsT[i]])

    def phase_final(self, b):
        P = self.P
        ar = self.ar
        ar.off = 0
        P.dma("sp", self.gvec[:], self.w["final_norm"].partition_broadcast(128), writes=[self.gvec])
        xt = [ar.alloc(128, [D], F32, "xf") for _ in range(3)]
        yt = [ar.alloc(128, [D], F32, "yf") for _ in range(2)]
        junk = ar.alloc(128, [D], BF16, "junkf")
        st = [ar.alloc(128, [4], F32, "stf") for _ in range(2)]
        for i in range(TT):
            x = xt[i % 3]
            y = yt[i % 2]
            s = st[i % 2]
            P.dma("sp", x[:], self.scr["xs"][i * 128:(i + 1) * 128, :], reads=[self.xsT[i]], writes=[x])
            P.op("act", lambda e, x=x, s=s: e.activation(out=junk[:], in_=x[:], func=AF.Square, accum_out=s[:, 0:1]), [x], [junk, s])
            P.op("dve", lambda e, s=s: e.tensor_scalar(out=s[:, 1:2], in0=s[:, 0:1], scalar1=1.0 / D, scalar2=EPS, op0=ALU.mult, op1=ALU.add), [s], [s])
            P.op("act", lambda e, s=s: e.activation(out=s[:, 2:3], in_=s[:, 1:2], func=AF.Sqrt), [s], [s])
            P.op("dve", lambda e, s=s: e.reciprocal(out=s[:, 3:4], in_=s[:, 2:3]), [s], [s])
            P.op("dve", lambda e, x=x, s=s, y=y: e.scalar_tensor_tensor(out=y[:], in0=x[:], scalar=s[:, 3:4], in1=self.gvec[:], op0=ALU.mult, op1=ALU.mult), [x, s, self.gvec], [y])
            P.dma("sp", self.y_out[b, i * 128:(i + 1) * 128, :], y[:], reads=[y])


_CACHE = {}


def kernel(**inputs):
    ncores = 8
    nb = 2
    B = Builder(nb, 2)
    nc = B.build()
    consts = host_consts()
    in_maps = []
    for c in range(ncores):
        m = {"x": np.ascontiguousarray(inputs["x"][c * nb:(c + 1) * nb])}
        for k in B.w:
            m[k] = np.ascontiguousarray(inputs[k])
        m.update(consts)
        in_maps.append(m)
    res = run_bass_kernel_spmd(nc, in_maps, core_ids=list(range(ncores)))
    return np.concatenate([np.asarray(r["y"]) for r in res.results], axis=0).astype(np.float32)
```
